# Optimizing a Trainium2 kernel written in Bass

```python
import math
import jax, jax.numpy as jnp
from jax import lax
import numpy as np

D_MODEL = 2048
BATCH = 8
SEQ = 2048
DEPTH = 1

A_HEADS = 8
A_HEAD_DIM = 128
A_WIDTH = A_HEADS * A_HEAD_DIM
MOBA_BLOCK = 256
MOBA_TOPK = 3
MOBA_Q_CHUNK = 16
REL_BUCKETS = 32
REL_MAX_EXACT = 16
REL_MAX_DIST = 128
B_HEADS = 8
B_KEY_DIM = 128
B_VAL_DIM = 128
B_FWIDTH = B_HEADS * B_KEY_DIM
B_IWIDTH = B_HEADS * B_VAL_DIM
HGRN_CHUNK = 64
D_FF = ((8 * D_MODEL // 3 + 255) // 256) * 256
PLE_DIM = 256
NORM_EPS = 1e-6
IN_SIZES = (A_WIDTH, A_WIDTH, A_WIDTH, B_FWIDTH, B_FWIDTH, B_IWIDTH, B_IWIDTH, D_MODEL, D_MODEL)
IN_WIDTH = sum(IN_SIZES)

kernel_name = "hybrid_moba_hgrn2_gated_block"


def rms_norm(x, gain):
    xf = x.astype(jnp.float32)
    y = xf * lax.rsqrt(jnp.mean(xf * xf, axis=-1, keepdims=True) + NORM_EPS)
    return (y * gain.astype(jnp.float32)).astype(x.dtype)


def t5_bucket(rel):
    n = jnp.maximum(rel, 0)
    nf = jnp.maximum(n, REL_MAX_EXACT).astype(jnp.float32)
    large = REL_MAX_EXACT + (jnp.log(nf / REL_MAX_EXACT) / math.log(REL_MAX_DIST / REL_MAX_EXACT)
                             * (REL_BUCKETS - REL_MAX_EXACT)).astype(jnp.int32)
    large = jnp.minimum(large, REL_BUCKETS - 1)
    return jnp.where(n < REL_MAX_EXACT, n, large)


def moba_attention(q, k, v, rel_bias):
    B, S, H, dh = q.shape
    nb = -(-S // MOBA_BLOCK)
    s_pad = nb * MOBA_BLOCK
    pad = ((0, 0), (0, s_pad - S), (0, 0), (0, 0))
    q = q.transpose(0, 2, 1, 3)
    kp = jnp.pad(k, pad).transpose(0, 2, 1, 3).reshape(B, H, nb, MOBA_BLOCK, dh)
    vp = jnp.pad(v, pad).transpose(0, 2, 1, 3).reshape(B, H, nb, MOBA_BLOCK, dh)
    scale = 1.0 / math.sqrt(dh)

    k_mean = jnp.mean(kp.astype(jnp.float32), axis=3)
    scores = jnp.einsum('bhsd,bhnd->bhsn', q.astype(jnp.float32), k_mean)
    q_blk = jnp.arange(S, dtype=jnp.int32) // MOBA_BLOCK
    past = jnp.arange(nb, dtype=jnp.int32)[None, :] < q_blk[:, None]
    scores = jnp.where(past[None, None], scores, -jnp.inf)
    own = jnp.broadcast_to(q_blk[None, None, :, None], (B, H, S, 1))
    n_sel = min(MOBA_TOPK, nb - 1)
    if n_sel > 0:
        _, top_idx = lax.top_k(scores, n_sel)
        top_idx = top_idx.astype(jnp.int32)
        sel = jnp.concatenate([top_idx, own], axis=-1)
        valid = jnp.concatenate([top_idx < own, jnp.ones_like(own, dtype=bool)], axis=-1)
    else:
        sel = own
        valid = jnp.ones_like(own, dtype=bool)
    J = sel.shape[-1]

    nqc = S // MOBA_Q_CHUNK
    def chunks(t):
        return jnp.moveaxis(t.reshape(B, H, nqc, MOBA_Q_CHUNK, *t.shape[3:]), 2, 0)
    xs = (chunks(q), chunks(sel), chunks(valid),
          jnp.arange(S, dtype=jnp.int32).reshape(nqc, MOBA_Q_CHUNK))
    bi = jnp.arange(B)[:, None, None, None]
    hi = jnp.arange(H)[None, :, None, None]
    hi5 = jnp.arange(H)[None, :, None, None, None]
    t_off = jnp.arange(MOBA_BLOCK, dtype=jnp.int32)

    def step(args):
        qc, sel_c, val_c, qpos = args
        kg = kp[bi, hi, sel_c]
        vg = vp[bi, hi, sel_c]
        logits = jnp.einsum('bhqd,bhqjtd->bhqjt', qc, kg).astype(jnp.float32) * scale
        kpos = sel_c[..., None] * MOBA_BLOCK + t_off
        rel = qpos[None, None, :, None, None] - kpos
        bias = rel_bias[t5_bucket(rel), hi5].astype(jnp.float32)
        mask = val_c[..., None] & (rel >= 0)
        logits = jnp.where(mask, logits + bias, -jnp.inf)
        probs = jax.nn.softmax(logits.reshape(B, H, MOBA_Q_CHUNK, J * MOBA_BLOCK), axis=-1)
        probs = probs.reshape(B, H, MOBA_Q_CHUNK, J, MOBA_BLOCK).astype(vg.dtype)
        return jnp.einsum('bhqjt,bhqjtd->bhqd', probs, vg)

    out = lax.map(step, xs)
    return out.transpose(1, 0, 3, 2, 4).reshape(B, S, H * dh)


def hgrn2_mixer(q, f_pre, i_in, g_out, lb, norm_gain):
    B, S, _ = q.shape
    dt = q.dtype
    f = lb.astype(jnp.float32) + (1.0 - lb.astype(jnp.float32)) * jax.nn.sigmoid(f_pre.astype(jnp.float32))
    log_f = jnp.log(f)
    k = 1.0 - f
    nc = S // HGRN_CHUNK
    def chunks(t, d):
        return t.astype(jnp.float32).reshape(B, nc, HGRN_CHUNK, B_HEADS, d).transpose(1, 0, 3, 2, 4)
    qc = chunks(q, B_KEY_DIM)
    kc = chunks(k, B_KEY_DIM)
    gc = chunks(log_f, B_KEY_DIM)
    vc = chunks(i_in, B_VAL_DIM)
    b = jnp.cumsum(gc, axis=3)
    b_last = b[:, :, :, -1:, :]
    q_dec = qc * jnp.exp(b)
    k_dec = kc * jnp.exp(-b)
    k_end = kc * jnp.exp(b_last - b)
    causal = jnp.tril(jnp.ones((HGRN_CHUNK, HGRN_CHUNK), jnp.float32))
    a = jnp.einsum('nbhtd,nbhsd->nbhts', q_dec, k_dec) * causal
    o_intra = jnp.einsum('nbhts,nbhsv->nbhtv', a, vc)

    def scan_fn(state, xs):
        q_d, k_e, v_c, decay = xs
        o_inter = jnp.einsum('bhtd,bhdv->bhtv', q_d, state)
        state = decay[:, :, 0, :, None] * state + jnp.einsum('bhsd,bhsv->bhdv', k_e, v_c)
        return state, o_inter

    s0 = jnp.zeros((B, B_HEADS, B_KEY_DIM, B_VAL_DIM), jnp.float32)
    _, o_inter = lax.scan(scan_fn, s0, (q_dec, k_end, vc, jnp.exp(b_last)))
    o = (o_intra + o_inter).transpose(1, 0, 3, 2, 4).reshape(B, S, B_HEADS, B_VAL_DIM)
    o = rms_norm(o.astype(dt), norm_gain.reshape(B_HEADS, B_VAL_DIM)).reshape(B, S, B_IWIDTH)
    return o * jax.nn.silu(g_out)


def setup_inputs(seed: int = 0) -> dict:
    key = jax.random.key(seed)
    ks = jax.random.split(key, 16)
    def w(k, shape, fan_in):
        return jax.random.normal(k, shape, jnp.float32) * fan_in ** -0.5
    def gain(k, shape):
        return 1.0 + 0.01 * jax.random.normal(k, shape, jnp.float32)
    return {
        "x": jax.random.normal(ks[0], (BATCH, SEQ, D_MODEL), jnp.float32),
        "p": jax.random.normal(ks[1], (DEPTH, BATCH, SEQ, PLE_DIM), jnp.float32),
        "norm_mix": gain(ks[2], (DEPTH, D_MODEL)),
        "w_in": w(ks[3], (DEPTH, D_MODEL, IN_WIDTH), D_MODEL),
        "hgrn_norm": gain(ks[4], (DEPTH, B_IWIDTH)),
        "w_proj_a": w(ks[5], (DEPTH, A_WIDTH, D_MODEL), A_WIDTH),
        "w_proj_b": w(ks[6], (DEPTH, B_IWIDTH, D_MODEL), B_IWIDTH),
        "w_out": w(ks[7], (DEPTH, D_MODEL, D_MODEL), D_MODEL),
        "norm_ffn": gain(ks[8], (DEPTH, D_MODEL)),
        "w_gate_up": w(ks[9], (DEPTH, D_MODEL, 2 * D_FF), D_MODEL),
        "w_down": w(ks[10], (DEPTH, D_FF, D_MODEL), D_FF),
        "w_ple": w(ks[11], (DEPTH, PLE_DIM, D_MODEL), PLE_DIM),
        "w_ple_gate": w(ks[12], (DEPTH, D_MODEL, D_MODEL), D_MODEL),
        "rel_bias": 0.1 * jax.random.normal(ks[13], (REL_BUCKETS, A_HEADS), jnp.float32),
        "hgrn_lb_logits": 0.1 * jax.random.normal(ks[14], (DEPTH + 1, B_FWIDTH), jnp.float32),
        "norm_final": gain(ks[15], (D_MODEL,)),
    }


def reference(x, p, norm_mix, w_in, hgrn_norm, w_proj_a, w_proj_b, w_out, norm_ffn, w_gate_up,
              w_down, w_ple, w_ple_gate, rel_bias, hgrn_lb_logits, norm_final):
    B, S, _ = x.shape
    split_points = list(np.cumsum(IN_SIZES)[:-1])
    lb_all = jnp.cumsum(jax.nn.softmax(hgrn_lb_logits.astype(jnp.float32), axis=0), axis=0)
    for i in range(DEPTH):
        h = rms_norm(x, norm_mix[i])
        proj = h @ w_in[i]
        qa, ka, va, qb, fb, ib, gb, gate_a, gate_b = jnp.split(proj, split_points, axis=-1)
        ya = moba_attention(qa.reshape(B, S, A_HEADS, A_HEAD_DIM),
                            ka.reshape(B, S, A_HEADS, A_HEAD_DIM),
                            va.reshape(B, S, A_HEADS, A_HEAD_DIM), rel_bias)
        yb = hgrn2_mixer(qb, fb, ib, gb, lb_all[i], hgrn_norm[i])
        mixed = jax.nn.sigmoid(gate_a) * (ya @ w_proj_a[i]) + jax.nn.sigmoid(gate_b) * (yb @ w_proj_b[i])
        x = x + mixed @ w_out[i]
        h = rms_norm(x, norm_ffn[i])
        gate, up = jnp.split(h @ w_gate_up[i], 2, axis=-1)
        x = x + (jax.nn.silu(gate) * up) @ w_down[i]
        x = x + jax.nn.sigmoid(x @ w_ple_gate[i]) * (p[i] @ w_ple[i])
    return rms_norm(x, norm_final)
```

```python
import math
import os
import contextlib
import numpy as np
import concourse.bass as bass
import concourse.mybir as mybir
from concourse.bass_utils import run_bass_kernel_spmd

AF = mybir.ActivationFunctionType
ALU = mybir.AluOpType
AX = mybir.AxisListType
F32 = mybir.dt.float32
BF16 = mybir.dt.bfloat16

S_LEN = 2048
D = 2048
NG = 4
GT = 512
DFF = 5632
BIG = 30000.0
NEG = -1.0e30
SCALE = 1.0 / math.sqrt(128.0)
EPS = 1e-6
ENGS = ("pe", "act", "dve", "pool", "sp")


class _Op:
    __slots__ = ("eng", "fn", "deps", "dma", "semval", "flag", "sigidx", "idx")


class Sched:
    def __init__(self, nc, stack):
        self.nc = nc
        self.stack = stack
        self.ops = []
        self.last_w = {}
        self.readers = {}
        self.dma_cnt = {}
        self.cnt = {e: 0 for e in ENGS}
        self.esem = {e: stack.enter_context(nc.semaphore("s_" + e)) for e in ENGS}
        self.dsem = {}
        self.nsem = 0

    def add(self, eng, fn, reads=(), writes=(), dma=None):
        op = _Op()
        op.eng, op.fn, op.dma, op.flag, op.sigidx = eng, fn, dma, False, 0
        op.idx = len(self.ops)
        deps = {}
        for r in reads:
            p = self.last_w.get(r)
            if p is not None:
                deps[p] = "raw"
            if isinstance(r, tuple) and r[0] == "ps":
                for p in self.readers.get(r, {}).values():
                    if p not in deps:
                        deps[p] = "rar"
        for w in writes:
            p = self.last_w.get(w)
            if p is not None and p not in deps:
                deps[p] = "waw"
            for p in self.readers.get(w, {}).values():
                if p not in deps:
                    deps[p] = "war"
        op.deps = deps
        if dma is not None:
            self.dma_cnt[dma] = self.dma_cnt.get(dma, 0) + 16
            op.semval = self.dma_cnt[dma]
            if dma not in self.dsem:
                self.dsem[dma] = self.stack.enter_context(self.nc.semaphore("d%d" % self.nsem))
                self.nsem += 1
        for r in reads:
            d = self.readers.setdefault(r, {})
            d[eng if dma is None else ("dma", op.idx)] = op.idx
        for w in writes:
            self.last_w[w] = op.idx
            self.readers[w] = {}
        self.ops.append(op)
        return op

    def emit(self):
        nc = self.nc
        ops = self.ops
        waits = []
        for op in ops:
            wl = []
            for p, kind in op.deps.items():
                P = ops[p]
                if P.dma is not None:
                    wl.append(("dma", P.dma, P.semval))
                elif P.eng == op.eng and op.dma is None:
                    if P.eng == "pe" or kind != "raw":
                        continue
                    P.flag = True
                    wl.append(("eng", p))
                else:
                    P.flag = True
                    wl.append(("eng", p))
            waits.append(wl)
        last = {}
        for op in ops:
            if op.dma is None and op.fn is not None:
                last[op.eng] = op
        for op in last.values():
            op.flag = True
        for op in ops:
            if op.flag:
                self.cnt[op.eng] += 1
                op.sigidx = self.cnt[op.eng]
        esem, dsem = self.esem, self.dsem
        endcnt = dict(self.cnt)
        enddma = dict(self.dma_cnt)
        with nc.Block() as block:
            def run(ename):
                def body(eng):
                    waited = {}
                    for op, wl in zip(ops, waits):
                        if op.eng != ename:
                            continue
                        need = {}
                        for w in wl:
                            if w[0] == "dma":
                                s, v = dsem[w[1]], w[2]
                            else:
                                P = ops[w[1]]
                                s, v = esem[P.eng], P.sigidx
                            if need.get(s, 0) < v:
                                need[s] = v
                        for s, v in need.items():
                            if waited.get(s, 0) < v:
                                eng.wait_ge(s, v)
                                waited[s] = v
                        ins = op.fn(eng)
                        if op.dma is not None:
                            ins.then_inc(dsem[op.dma], 16)
                        elif op.flag:
                            ins.then_inc(esem[ename], 1)
                    for e2 in ENGS:
                        if endcnt[e2] > 0:
                            eng.wait_ge(esem[e2], endcnt[e2])
                    for k, v in enddma.items():
                        eng.wait_ge(dsem[k], v)
                return body

            block.tensor(run("pe"))
            block.scalar(run("act"))
            block.vector(run("dve"))
            block.gpsimd(run("pool"))
            block.sync(run("sp"))
        self.ops = []
        self.last_w = {}
        self.readers = {}


class Banks:
    def __init__(self, n):
        self.free = list(range(n))

    def get(self):
        return self.free.pop(0)

    def put(self, b):
        self.free.append(b)


class Prog:
    def __init__(self, debug=False, stop=9):
        self.debug = debug
        self.stop = stop
        self.nc = bass.Bass("TRN2", target_bir_lowering=False)

    def mm(self, out, lhsT, rhs, start, stop, reads, writes):
        self.S.add("pe", lambda e: e.matmul(out, lhsT=lhsT, rhs=rhs, start=start, stop=stop), reads, writes)

    def tr(self, out, in_, ident, reads, writes):
        self.S.add("pe", lambda e: e.transpose(out=out, in_=in_, identity=ident), reads, writes)

    def act(self, out, in_, func, reads, writes, **kw):
        self.S.add("act", lambda e: e.activation(out=out, in_=in_, func=func, **kw), reads, writes)

    def tt(self, out, in0, in1, op, reads, writes):
        self.S.add("dve", lambda e: e.tensor_tensor(out=out, in0=in0, in1=in1, op=op), reads, writes)

    def ts(self, out, in0, s1, s2, op0, op1, reads, writes):
        self.S.add("dve", lambda e: e.tensor_scalar(out=out, in0=in0, scalar1=s1, scalar2=s2, op0=op0, op1=op1), reads, writes)

    def stt(self, out, in0, scalar, in1, op0, op1, reads, writes):
        self.S.add("dve", lambda e: e.scalar_tensor_tensor(out=out, in0=in0, scalar=scalar, in1=in1, op0=op0, op1=op1), reads, writes)

    def cp(self, eng, out, in_, reads, writes):
        if eng == "act":
            self.S.add("act", lambda e: e.activation(out=out, in_=in_, func=AF.Copy), reads, writes)
        else:
            self.S.add("dve", lambda e: e.tensor_copy(out=out, in_=in_), reads, writes)

    def recip(self, out, in_, reads, writes):
        self.S.add("dve", lambda e: e.reciprocal(out=out, in_=in_), reads, writes)

    def dma(self, q, out, in_, reads, writes, key):
        self.S.add(q, lambda e: e.dma_start(out=out, in_=in_), reads, writes, dma=key)

    def load_panel(self, wd, k0, KC, c0, W):
        s = self.wnext % len(self.wring)
        self.wnext += 1
        src = wd[k0 * 128:(k0 + KC) * 128, c0:c0 + W].rearrange("(kc p) n -> p kc n", p=128)
        dst = self.wring[s][:, 0:KC, 0:W]
        self.dma("pool", dst, src, [], [("w", s)], ("w", s))
        return s

    def gemm(self, bank, slot, KC, oc, rhs_fn, rhs_keys):
        for kc in range(KC):
            self.mm(self.ps[bank][:, :], self.wring[slot][:, kc, oc * 128:(oc + 1) * 128], rhs_fn(kc),
                    kc == 0, kc == KC - 1, [("w", slot)] + rhs_keys(kc), [("ps", bank)])

    def load_xT(self, g, xT, xst):
        for ti in range(4):
            i = 4 * g + ti
            b = self.xcnt % 2
            self.xcnt += 1
            self.dma("sp", xst[b][:], self.x_d[i * 128:(i + 1) * 128, :], [], [("xst", b)], ("xst", b))
            for q4 in range(4):
                bank = self.pb.get()
                for j in range(4):
                    kc = q4 * 4 + j
                    self.tr(self.ps[bank][:, j * 128:(j + 1) * 128], xst[b][:, kc * 128:(kc + 1) * 128], self.ident_f,
                            [("xst", b), "cst"], [("ps", bank)])
                self.cp("act" if q4 % 2 == 0 else "dve",
                        xT[:, q4 * 4:q4 * 4 + 4, ti * 128:(ti + 1) * 128],
                        self.ps[bank][:, :].rearrange("p (j t) -> p j t", j=4),
                        [("ps", bank)], [("xT", q4)])
                self.pb.put(bank)

    def rmsnorm_fm(self, src, gain, dst_fn, dstkey, nfeat=2048.0):
        bank = self.pb.get()
        for kc in range(16):
            b = self.sqc % 2
            self.sqc += 1
            self.act(self.sq[b][:], src[:, kc, :], AF.Square, [("xT", kc // 4)], [("sq", b)])
            self.mm(self.ps[bank][:, :], self.ones_b[:], self.sq[b][:], kc == 0, kc == 15,
                    [("sq", b), "ones"], [("ps", bank)])
        self.act(self.rt[:], self.ps[bank][:, :], AF.Sqrt, [("ps", bank), "eps"], ["rt"], scale=1.0 / nfeat, bias=self.eps[:, 0:1])
        self.pb.put(bank)
        self.recip(self.rstd[:], self.rt[:], ["rt"], ["rstd"])
        for kc in range(16):
            self.stt(dst_fn(kc), src[:, kc, :], gain[:, kc:kc + 1], self.rstd[:], ALU.mult, ALU.mult,
                     [("xT", kc // 4), "rstd", "vec"], [dstkey(kc)])

    def build(self):
        nc = self.nc
        dti = lambda name, shape: nc.dram_tensor(name, shape, F32, kind="ExternalInput").ap()
        self.x_d = dti("x", [S_LEN, D])
        self.p_d = dti("p", [S_LEN, 256])
        self.w_in = dti("w_in", [D, 11264])
        self.w_pa = dti("w_pa", [1024, D])
        self.w_pb = dti("w_pb", [1024, D])
        self.w_out = dti("w_out", [D, D])
        self.w_gu = dti("w_gu", [D, 2 * DFF])
        self.w_dn = dti("w_dn", [DFF, D])
        self.w_ple = dti("w_ple", [256, D])
        self.w_pg = dti("w_pg", [D, D])
        cst_d = dti("cst", [128, 1024])
        vec_d = dti("vec", [128, 72])
        oh_d = dti("oh", [32, 512])
        c8_d = dti("c8", [8, 1408])
        rb_d = dti("rb", [32, 8])
        self.out_d = nc.dram_tensor("out", [S_LEN, D], F32, kind="ExternalOutput").ap()
        tz = nc.dram_tensor("tz", [8, 128, 384], F32)
        if self.debug:
            self.dbg_hT = nc.dram_tensor("dbg_hT", [128, 16 * 2048], BF16, kind="ExternalOutput").ap()
            self.dbg_ya = nc.dram_tensor("dbg_ya", [128, 8 * 2048], BF16, kind="ExternalOutput").ap()
            self.dbg_yb = nc.dram_tensor("dbg_yb", [128, 8 * 2048], BF16, kind="ExternalOutput").ap()
            self.dbg_bias = nc.dram_tensor("dbg_bias", [128, 8 * 2 * 128], BF16, kind="ExternalOutput").ap()

        with contextlib.ExitStack() as G:
            T = lambda st, name, shape, d: st.enter_context(nc.sbuf_tensor(name, shape, d))
            self.S = Sched(nc, G)
            self.ps = [G.enter_context(nc.psum_tensor("ps%d" % i, [128, 512], F32)) for i in range(8)]
            self.pb = Banks(8)
            self.xcnt = 0
            self.sqc = 0
            self.wnext = 0
            cst = T(G, "cst_sb", [128, 1024], F32)
            vec = T(G, "vec_sb", [128, 72], F32)
            self.ident_f = cst[:, 0:128]
            pastm = cst[:, 128:256]
            ownm = cst[:, 256:384]
            maskCT = cst[:, 384:512]
            resetm = cst[:, 512:1024]
            g_mix, g_ffn, g_fin = vec[:, 0:16], vec[:, 16:32], vec[:, 32:48]
            hg = vec[:, 48:56]
            self.ident_b = T(G, "ident_b", [128, 128], BF16)
            self.ones_b = T(G, "ones_b", [128, 128], BF16)
            eselb = T(G, "eselb", [8, 1024], BF16)
            biasT = T(G, "biasT", [128, 8, 2, 128], BF16)
            c31 = T(G, "c31", [128, 8], F32)
            lb = T(G, "lb", [128, 8], F32)
            omlb = T(G, "omlb", [128, 8], F32)
            dl = T(G, "dl", [128, 8], F32)
            self.eps = T(G, "eps", [128, 1], F32)
            self.sq = [T(G, "sq%d" % i, [128, 512], BF16) for i in range(2)]
            self.rt = T(G, "rt", [128, 512], F32)
            self.rstd = T(G, "rstd", [128, 512], F32)
            yaT = T(G, "yaT", [128, 8, 2048], BF16)
            ybT = T(G, "ybT", [128, 8, 2048], BF16)
            ident_b, ones_b = self.ident_b, self.ones_b
            S = self.S

            with contextlib.ExitStack() as H:
                hT = T(H, "hT", [128, 16, 2048], BF16)
                with contextlib.ExitStack() as P1:
                    oh = T(P1, "oh_sb", [32, 512], F32)
                    c8 = T(P1, "c8_sb", [8, 1408], F32)
                    rb = T(P1, "rb_sb", [32, 8], F32)
                    v_sb = T(P1, "v_sb", [8, 384], F32)
                    vp = T(P1, "vp", [8, 384], F32)
                    btf = T(P1, "btf", [128, 8, 2, 128], F32)
                    self.dma("sp", cst[:], cst_d, [], ["cst"], "c_cst")
                    self.dma("sp", vec[:], vec_d, [], ["vec"], "c_vec")
                    self.dma("sp", oh[:], oh_d, [], ["oh"], "c_oh")
                    self.dma("sp", c8[:], c8_d, [], ["c8"], "c_c8")
                    self.dma("sp", rb[:], rb_d, [], ["rb"], "c_rb")
                    self.cp("dve", ident_b[:], self.ident_f, ["cst"], ["identb"])
                    S.add("dve", lambda e: e.memset(ones_b[:], 1.0), [], ["ones"])
                    S.add("dve", lambda e: e.memset(self.eps[:], EPS), [], ["eps"])
                    self.cp("dve", eselb[:], c8[:, 384:1408], ["c8"], ["eselb"])
                    self.tt(dl[:], vec[:, 56:64], vec[:, 64:72], ALU.subtract, ["vec"], ["dl"])
                    self.act(lb[:], dl[:], AF.Sigmoid, ["dl"], ["lb"])
                    self.act(omlb[:], dl[:], AF.Sigmoid, ["dl"], ["omlb"], scale=-1.0)
                    b1 = self.pb.get()
                    self.mm(self.ps[b1][0:8, 0:384], rb[:, :], oh[:, 0:384], True, True, ["rb", "oh"], [("ps", b1)])
                    self.cp("act", v_sb[:], self.ps[b1][0:8, 0:384], [("ps", b1)], ["v_sb"])
                    self.pb.put(b1)
                    b2 = self.pb.get()
                    self.mm(self.ps[b2][:, 0:8], oh[:, 384:512], rb[:, :], True, True, ["rb", "oh"], [("ps", b2)])
                    self.cp("act", c31[:], self.ps[b2][:, 0:8], [("ps", b2)], ["c31"])
                    self.pb.put(b2)
                    self.stt(vp[:], v_sb[:], v_sb[:, 382:383], c8[:, 0:384], ALU.subtract, ALU.add, ["v_sb", "c8"], ["vp"])
                    self.dma("sp", tz.ap(), vp[:].unsqueeze(1).broadcast_to([8, 128, 384]), ["vp"], ["tz"], "c_tzw")
                    for ty, off in ((0, 127), (1, 255)):
                        src = bass.AP(tz, off, [[383, 128], [128 * 384, 8], [1, 128]])
                        self.dma("sp", btf[:, :, ty, :], src, ["tz"], [("btf", ty)], "c_tzr%d" % ty)
                    self.cp("dve", biasT[:], btf[:], [("btf", 0), ("btf", 1)], ["biasT"])
                    if self.debug:
                        self.dma("sp", self.dbg_bias, biasT[:].rearrange("p a b c -> p (a b c)"), ["biasT"], [], "dbgb")
                    S.emit()
                    if self.stop == 0:
                        return nc
                with contextlib.ExitStack() as P1:
                    xT = T(P1, "xT1", [128, 16, 512], F32)
                    xst = [T(P1, "xst%d" % i, [128, 2048], F32) for i in range(2)]
                    for g in range(NG):
                        self.load_xT(g, xT, xst)
                        self.rmsnorm_fm(xT, g_mix, lambda kc, g=g: hT[:, kc, g * GT:(g + 1) * GT], lambda kc, g=g: ("hT", g))
                    if self.debug:
                        self.dma("sp", self.dbg_hT, hT[:].rearrange("p a b -> p (a b)"), [("hT", g) for g in range(4)], [], "dbg0")
                    S.emit()
                    if self.stop == 1:
                        return nc

                with contextlib.ExitStack() as A:
                    self.wring = [T(A, "wrA%d" % i, [128, 16, 128], BF16) for i in range(8)]
                    self.wnext = 0
                    hrhs = lambda g: (lambda kc: hT[:, kc, g * GT:(g + 1) * GT])
                    hkeys = lambda g: (lambda kc: [("hT", g)])
                    AM = contextlib.ExitStack()
                    AM.__enter__()
                    qT = [T(AM, "qT%d" % i, [128, 512], BF16) for i in range(2)]
                    kT = T(AM, "kT", [128, 2048], BF16)
                    V = T(AM, "V", [128, 16, 128], BF16)
                    vTg = T(AM, "vTg", [128, 512], BF16)
                    kms = T(AM, "kms", [128, 8], F32)
                    kmsb = T(AM, "kmsb", [128, 8], BF16)
                    sc = T(AM, "sc", [128, 4, 8], F32)
                    top8 = T(AM, "top8", [128, 4, 8], F32)
                    sel = T(AM, "sel", [128, 4, 8], F32)
                    selm1 = T(AM, "selm1", [128, 4, 8], BF16)
                    selT = [T(AM, "selT%d" % i, [8, 512], BF16) for i in range(2)]
                    pT = [T(AM, "pT%d" % i, [128, 512], BF16) for i in range(3)]
                    rs = T(AM, "rs", [128, 512], F32)
                    S.add("dve", lambda e: e.memset(kms[:], 0.0), [], ["kms"])
                    self.cp("dve", kmsb[:], kms[:], ["kms"], ["kmsb"])
                    qc = [0]
                    pc = [0]
                    sc3, sel3 = sc[:], sel[:]

                    CUT = int(os.environ.get("MK_CUT", "99"))
                    for h in range(int(os.environ.get("MK_NH", "8"))):
                        s_q = self.load_panel(self.w_in, 0, 16, h * 128, 128)
                        s_k = self.load_panel(self.w_in, 0, 16, 1024 + h * 128, 128)
                        s_v = self.load_panel(self.w_in, 0, 16, 2048 + h * 128, 128)
                        for g in range(NG):
                            qb = qc[0] % 2
                            qc[0] += 1
                            bank = self.pb.get()
                            self.gemm(bank, s_q, 16, 0, hrhs(g), hkeys(g))
                            self.act(qT[qb][:], self.ps[bank][:, :], AF.Copy, [("ps", bank)], [("qT", qb)], scale=SCALE)
                            self.pb.put(bank)
                            if CUT <= 1:
                                continue
                            bank = self.pb.get()
                            self.gemm(bank, s_k, 16, 0, hrhs(g), hkeys(g))
                            self.act(kT[:, g * GT:(g + 1) * GT], self.ps[bank][:, :], AF.Copy, [("ps", bank)], [("kT", g)])
                            S.add("dve", lambda e, bank=bank, g=g: e.tensor_reduce(
                                out=kms[:, 2 * g:2 * g + 2], in_=self.ps[bank][:, :].rearrange("p (b t) -> p b t", b=2),
                                axis=AX.X, op=ALU.add), [("ps", bank)], ["kms"])
                            self.pb.put(bank)
                            self.cp("dve", kmsb[:, 2 * g:2 * g + 2], kms[:, 2 * g:2 * g + 2], ["kms"], ["kmsb"])
                            if CUT <= 2:
                                continue
                            bank = self.pb.get()
                            self.gemm(bank, s_v, 16, 0, hrhs(g), hkeys(g))
                            self.act(vTg[:], self.ps[bank][:, :], AF.Copy, [("ps", bank)], ["vTg"])
                            self.pb.put(bank)
                            bank = self.pb.get()
                            pbv = self.ps[bank][:, :].bitcast(BF16)
                            for ti in range(4):
                                self.tr(pbv[:, ti * 128:(ti + 1) * 128], vTg[:, ti * 128:(ti + 1) * 128], ident_b[:],
                                        ["vTg", "identb"], [("ps", bank)])
                            self.cp("dve", V[:, 4 * g:4 * g + 4, :], pbv[:, 0:512].rearrange("p (j t) -> p j t", j=4),
                                    [("ps", bank)], [("V", g)])
                            self.pb.put(bank)
                            if CUT <= 3:
                                continue
                            bank = self.pb.get()
                            for ti in range(4):
                                self.mm(self.ps[bank][:, ti * 8:(ti + 1) * 8], qT[qb][:, ti * 128:(ti + 1) * 128], kmsb[:, 0:8],
                                        True, True, [("qT", qb), "kmsb"], [("ps", bank)])
                            self.tt(sc[:].rearrange("p a b -> p (a b)"), self.ps[bank][:, 0:32], pastm[:, g * 32:(g + 1) * 32], ALU.add,
                                    [("ps", bank), "cst"], ["sc"])
                            self.pb.put(bank)
                            for ti in range(4):
                                S.add("dve", lambda e, ti=ti: e.max(out=top8[:, ti, :], in_=sc[:, ti, :]), ["sc"], ["top8"])
                            self.tt(sel3, sc3, top8[:, :, 2:3].broadcast_to([128, 4, 8]), ALU.is_ge, ["sc", "top8"], ["sel"])
                            self.stt(selm1[:], sel3, -1.0, ownm[:, g * 32:(g + 1) * 32].rearrange("p (a b) -> p a b", a=4),
                                     ALU.add, ALU.max, ["sel", "cst"], ["selm1"])
                            if CUT <= 4:
                                continue
                            bank = self.pb.get()
                            pbv = self.ps[bank][:, :].bitcast(BF16)
                            for ti in range(4):
                                self.tr(pbv[0:8, ti * 128:(ti + 1) * 128], selm1[:, ti, :], ident_b[:], ["selm1", "identb"], [("ps", bank)])
                            sb = qb
                            self.cp("act", selT[sb][0:8, :], pbv[0:8, 0:512], [("ps", bank)], [("selT", sb)])
                            self.pb.put(bank)
                            if CUT <= 5:
                                continue
                            bo = self.pb.get()
                            bs = self.pb.get()
                            nj = 4 * g + 4

                            def emitS(j, g=g, h=h, qb=qb, sb=sb):
                                c0 = max(0, j - 4 * g) * 128
                                bk = self.pb.get()
                                items = [(kT[:, j * 128:(j + 1) * 128], qT[qb][:, c0:512], c0, 512, [("kT", j // 4), ("qT", qb)])]
                                if j < 4 * g + 2:
                                    n = j // 2
                                    items.append((eselb[0:8, n * 128:(n + 1) * 128], selT[sb][0:8, c0:512], c0, 512, ["eselb", ("selT", sb)]))
                                if j >= 4 * g:
                                    items.append((ident_b[:], biasT[:, h, 0, :], c0, c0 + 128, ["identb", "biasT"]))
                                if 4 * g <= j + 1 <= 4 * g + 3:
                                    cs = (j + 1 - 4 * g) * 128
                                    items.append((ident_b[:], biasT[:, h, 1, :], cs, cs + 128, ["identb", "biasT"]))
                                for n_, (l, r, a, b_, rd) in enumerate(items):
                                    self.mm(self.ps[bk][:, a:b_], l, r, n_ == 0, n_ == len(items) - 1, rd, [("ps", bk)])
                                pbi = pc[0] % 3
                                pc[0] += 1
                                self.act(pT[pbi][:, c0:512], self.ps[bk][:, c0:512], AF.Exp, [("ps", bk), "c31"], [("pT", pbi)],
                                         bias=c31[:, h:h + 1])
                                self.pb.put(bk)
                                return pbi, c0

                            def emitPV(j, pbi, c0, bo=bo, bs=bs, nj=nj):
                                self.mm(self.ps[bo][:, c0:512], V[:, j, :], pT[pbi][:, c0:512], j == 0, j == nj - 1,
                                        [("V", j // 4), ("pT", pbi)], [("ps", bo)])
                                self.mm(self.ps[bs][:, c0:512], ones_b[:], pT[pbi][:, c0:512], j == 0, j == nj - 1,
                                        ["ones", ("pT", pbi)], [("ps", bs)])

                            prev = emitS(0)
                            for j in range(nj):
                                nxt = emitS(j + 1) if j + 1 < nj else None
                                emitPV(j, *prev)
                                prev = nxt
                            self.recip(rs[:], self.ps[bs][:, :], [("ps", bs)], ["rs"])
                            self.tt(yaT[:, h, g * GT:(g + 1) * GT], self.ps[bo][:, :], rs[:], ALU.mult, [("ps", bo), "rs"], [("ya", h)])
                            self.pb.put(bo)
                            self.pb.put(bs)

                    if self.debug:
                        self.dma("sp", self.dbg_ya, yaT[:].rearrange("p a b -> p (a b)"), [("ya", h) for h in range(8)], [], "dbg1")
                    S.emit()
                    AM.__exit__(None, None, None)
                    if self.stop == 2:
                        return nc
                    AH = contextlib.ExitStack()
                    AH.__enter__()
                    t1 = T(AH, "t1", [128, 512], F32)
                    t2 = T(AH, "t2", [128, 512], F32)
                    t3 = T(AH, "t3", [128, 512], F32)
                    t4 = T(AH, "t4", [128, 512], F32)
                    kd = T(AH, "kd", [128, 512], BF16)
                    ke = T(AH, "ke", [128, 512], BF16)
                    qd = T(AH, "qd", [128, 512], BF16)
                    ibT = T(AH, "ibT", [128, 512], BF16)
                    sg = T(AH, "sg", [128, 512], F32)
                    dec = T(AH, "dec", [128, 8], F32)
                    ke_tm = T(AH, "ke_tm", [128, 4, 128], BF16)
                    v_tm = T(AH, "v_tm", [128, 4, 128], BF16)
                    Sf = [T(AH, "Sf%d" % i, [128, 128], F32) for i in range(2)]
                    Sbf = T(AH, "Sbf", [128, 9, 128], BF16)
                    aT = T(AH, "aT", [128, 4, 128], BF16)
                    sqb = T(AH, "sqb", [128, 512], BF16)
                    tmp = T(AH, "tmpA", [128, 512], F32)
                    v3 = lambda t: t[:].rearrange("p (c t) -> p c t", t=64)
                    for h in range(8):
                        s_q = self.load_panel(self.w_in, 0, 16, 3072 + h * 128, 128)
                        s_f = self.load_panel(self.w_in, 0, 16, 4096 + h * 128, 128)
                        s_i = self.load_panel(self.w_in, 0, 16, 5120 + h * 128, 128)
                        s_g = self.load_panel(self.w_in, 0, 16, 6144 + h * 128, 128)
                        S.add("dve", lambda e: e.memset(Sf[0][:], 0.0), [], [("Sf", 0)])
                        S.add("dve", lambda e: e.memset(Sbf[:, 0, :], 0.0), [], [("Sbf", 0)])
                        for g in range(NG):
                            if g > 0:
                                self.cp("act", Sbf[:, 0, :], Sbf[:, 8, :], [("Sbf", 8)], [("Sbf", 0)])
                            bank = self.pb.get()
                            self.gemm(bank, s_f, 16, 0, hrhs(g), hkeys(g))
                            self.act(t1[:], self.ps[bank][:, :], AF.Sigmoid, [("ps", bank)], ["t1"])
                            self.pb.put(bank)
                            self.ts(t1[:], t1[:], omlb[:, h:h + 1], lb[:, h:h + 1], ALU.mult, ALU.add, ["t1", "lb", "omlb"], ["t1"])
                            self.ts(t2[:], t1[:], -1.0, 1.0, ALU.mult, ALU.add, ["t1"], ["t2"])
                            self.act(t3[:], t1[:], AF.Ln, ["t1"], ["t3"])
                            S.add("dve", lambda e: e.tensor_tensor_scan(out=t4[:], data0=resetm, data1=t3[:], initial=0.0,
                                                                        op0=ALU.mult, op1=ALU.add), ["t3", "cst"], ["t4"])
                            self.act(t1[:], t4[:], AF.Exp, ["t4"], ["t1"])
                            self.act(t3[:], t4[:], AF.Exp, ["t4"], ["t3"], scale=-1.0)
                            self.tt(t2[:], t2[:], t3[:], ALU.mult, ["t2", "t3"], ["t2"])
                            self.cp("act", kd[:], t2[:], ["t2"], ["kd"])
                            self.tt(v3(ke), v3(t2), v3(t1)[:, :, 63:64].broadcast_to([128, 8, 64]), ALU.mult, ["t2", "t1"], ["ke"])
                            self.cp("dve", dec[:], v3(t1)[:, :, 63], ["t1"], ["dec"])
                            bank = self.pb.get()
                            self.gemm(bank, s_q, 16, 0, hrhs(g), hkeys(g))
                            self.tt(qd[:], self.ps[bank][:, :], t1[:], ALU.mult, [("ps", bank), "t1"], ["qd"])
                            self.pb.put(bank)
                            bank = self.pb.get()
                            self.gemm(bank, s_i, 16, 0, hrhs(g), hkeys(g))
                            self.act(ibT[:], self.ps[bank][:, :], AF.Copy, [("ps", bank)], ["ibT"])
                            self.pb.put(bank)
                            bank = self.pb.get()
                            self.gemm(bank, s_g, 16, 0, hrhs(g), hkeys(g))
                            self.act(sg[:], self.ps[bank][:, :], AF.Silu, [("ps", bank)], ["sg"])
                            self.pb.put(bank)
                            for srcT, dstT, key_s, key_d, eng in ((ke, ke_tm, "ke", "ke_tm", "dve"), (ibT, v_tm, "ibT", "v_tm", "act")):
                                bank = self.pb.get()
                                pbv = self.ps[bank][:, :].bitcast(BF16)
                                for ti in range(4):
                                    self.tr(pbv[:, ti * 128:(ti + 1) * 128], srcT[:, ti * 128:(ti + 1) * 128], ident_b[:],
                                            [key_s, "identb"], [("ps", bank)])
                                self.cp(eng, dstT[:], pbv[:, 0:512].rearrange("p (j t) -> p j t", j=4), [("ps", bank)], [key_d])
                                self.pb.put(bank)
                            bu = [self.pb.get(), self.pb.get()]
                            ubank = lambda c: bu[c % 2]
                            ucol = lambda c: slice((c // 2) * 128, (c // 2 + 1) * 128)
                            for c in range(8):
                                ti, r0 = c // 2, (c % 2) * 64
                                self.mm(self.ps[ubank(c)][:, ucol(c)], ke_tm[r0:r0 + 64, ti, :], v_tm[r0:r0 + 64, ti, :],
                                        True, True, ["ke_tm", "v_tm"], [("ps", ubank(c))])
                            for c in range(8):
                                cg = 8 * g + c
                                self.stt(Sf[(cg + 1) % 2][:], Sf[cg % 2][:], dec[:, c:c + 1],
                                         self.ps[ubank(c)][:, ucol(c)], ALU.mult, ALU.add,
                                         [("Sf", cg % 2), "dec", ("ps", ubank(c))], [("Sf", (cg + 1) % 2)])
                                self.cp("act", Sbf[:, c + 1, :], Sf[(cg + 1) % 2][:], [("Sf", (cg + 1) % 2)], [("Sbf", c + 1)])
                            self.pb.put(bu[0])
                            self.pb.put(bu[1])
                            bank = self.pb.get()
                            for ti in range(4):
                                self.mm(self.ps[bank][:, ti * 128:(ti + 1) * 128], kd[:, ti * 128:(ti + 1) * 128], qd[:, ti * 128:(ti + 1) * 128],
                                        True, True, ["kd", "qd"], [("ps", bank)])
                            self.tt(aT[:], self.ps[bank][:, :].rearrange("p (j t) -> p j t", j=4),
                                    maskCT.unsqueeze(1).broadcast_to([128, 4, 128]), ALU.mult, [("ps", bank), "cst"], ["aT"])
                            self.pb.put(bank)
                            bank = self.pb.get()
                            for ti in range(4):
                                self.mm(self.ps[bank][:, ti * 128:(ti + 1) * 128], v_tm[:, ti, :], aT[:, ti, :], True, False,
                                        ["v_tm", "aT"], [("ps", bank)])
                                for half in range(2):
                                    c = 2 * ti + half
                                    cg = 8 * g + c
                                    self.mm(self.ps[bank][:, c * 64:(c + 1) * 64], Sbf[:, c, :], qd[:, c * 64:(c + 1) * 64], False, half == 1,
                                            [("Sbf", c), "qd"], [("ps", bank)])
                            self.act(sqb[:], self.ps[bank][:, :], AF.Square, [("ps", bank)], ["sqb"])
                            bank2 = self.pb.get()
                            self.mm(self.ps[bank2][:, :], ones_b[:], sqb[:], True, True, ["sqb", "ones"], [("ps", bank2)])
                            self.act(self.rt[:], self.ps[bank2][:, :], AF.Sqrt, [("ps", bank2), "eps"], ["rt"], scale=1.0 / 128.0, bias=self.eps[:, 0:1])
                            self.pb.put(bank2)
                            self.recip(self.rstd[:], self.rt[:], ["rt"], ["rstd"])
                            self.stt(tmp[:], self.ps[bank][:, :], hg[:, h:h + 1], self.rstd[:], ALU.mult, ALU.mult,
                                     [("ps", bank), "rstd", "vec"], ["tmpA"])
                            self.pb.put(bank)
                            self.tt(ybT[:, h, g * GT:(g + 1) * GT], tmp[:], sg[:], ALU.mult, ["tmpA", "sg"], [("yb", h)])
                    if self.debug:
                        self.dma("sp", self.dbg_yb, ybT[:].rearrange("p a b -> p (a b)"), [("yb", h) for h in range(8)], [], "dbg2")
                    S.emit()
                    AH.__exit__(None, None, None)
                    if self.stop == 3:
                        return nc

            with contextlib.ExitStack() as C:
                self.wring = [T(C, "wrC%d" % i, [128, 16, 256], BF16) for i in range(4)]
                self.wnext = 0
                xT = T(C, "xTc", [128, 16, 512], F32)
                hbuf = T(C, "hbuf", [128, 16, 512], BF16)
                mbuf = T(C, "mbuf", [128, 16, 512], BF16)
                xst = [T(C, "xsc%d" % i, [128, 2048], F32) for i in range(2)]
                s1 = T(C, "s1", [128, 512], F32)
                s2 = T(C, "s2", [128, 512], F32)
                s3 = T(C, "s3", [128, 512], F32)
                pst = T(C, "pst", [128, 4, 256], F32)
                pTg = T(C, "pTg", [128, 2, 512], BF16)
                xkey = lambda oc: ("xT", oc // 4)
                ocount = 0
                for g in range(NG):
                    tok = slice(g * GT, (g + 1) * GT)
                    self.load_xT(g, xT, xst)
                    self.rmsnorm_fm(xT, g_mix, lambda kc: hbuf[:, kc, :], lambda kc: ("hbuf", kc // 4))
                    hb_rhs = lambda kc: hbuf[:, kc, :]
                    hb_keys = lambda kc: [("hbuf", kc // 4)]
                    for op_ in range(8):
                        s_ga = self.load_panel(self.w_in, 0, 16, 7168 + op_ * 256, 256)
                        s_gb = self.load_panel(self.w_in, 0, 16, 9216 + op_ * 256, 256)
                        s_pa = self.load_panel(self.w_pa, 0, 8, op_ * 256, 256)
                        s_pb = self.load_panel(self.w_pb, 0, 8, op_ * 256, 256)
                        for o2 in range(2):
                            oc = op_ * 2 + o2
                            bga = self.pb.get()
                            self.gemm(bga, s_ga, 16, o2, hb_rhs, hb_keys)
                            self.act(s1[:], self.ps[bga][:, :], AF.Sigmoid, [("ps", bga)], ["s1"])
                            self.pb.put(bga)
                            bgb = self.pb.get()
                            self.gemm(bgb, s_gb, 16, o2, hb_rhs, hb_keys)
                            self.act(s2[:], self.ps[bgb][:, :], AF.Sigmoid, [("ps", bgb)], ["s2"])
                            self.pb.put(bgb)
                            bpa = self.pb.get()
                            self.gemm(bpa, s_pa, 8, o2, lambda kc: yaT[:, kc, tok], lambda kc: [("ya", kc)])
                            self.tt(s1[:], self.ps[bpa][:, :], s1[:], ALU.mult, [("ps", bpa), "s1"], ["s1"])
                            self.pb.put(bpa)
                            bpb = self.pb.get()
                            self.gemm(bpb, s_pb, 8, o2, lambda kc: ybT[:, kc, tok], lambda kc: [("yb", kc)])
                            self.tt(s2[:], self.ps[bpb][:, :], s2[:], ALU.mult, [("ps", bpb), "s2"], ["s2"])
                            self.pb.put(bpb)
                            self.tt(mbuf[:, oc, :], s1[:], s2[:], ALU.add, ["s1", "s2"], [("mbuf", oc)])
                    for op_ in range(8):
                        s_w = self.load_panel(self.w_out, 0, 16, op_ * 256, 256)
                        for o2 in range(2):
                            oc = op_ * 2 + o2
                            bank = self.pb.get()
                            self.gemm(bank, s_w, 16, o2, lambda kc: mbuf[:, kc, :], lambda kc: [("mbuf", kc)])
                            self.tt(xT[:, oc, :], xT[:, oc, :], self.ps[bank][:, :], ALU.add, [xkey(oc), ("ps", bank)], [xkey(oc)])
                            self.pb.put(bank)
                    self.rmsnorm_fm(xT, g_ffn, lambda kc: hbuf[:, kc, :], lambda kc: ("hbuf", kc // 4))
                    blk0 = 0
                    for nblk in (16, 16, 12):
                        for pr in range(nblk // 2):
                            cj = blk0 + 2 * pr
                            s_gt = self.load_panel(self.w_gu, 0, 16, cj * 128, 256)
                            s_up = self.load_panel(self.w_gu, 0, 16, DFF + cj * 128, 256)
                            for o2 in range(2):
                                jl = 2 * pr + o2
                                bg_ = self.pb.get()
                                self.gemm(bg_, s_gt, 16, o2, hb_rhs, hb_keys)
                                self.act(s3[:], self.ps[bg_][:, :], AF.Silu, [("ps", bg_)], ["s3"])
                                self.pb.put(bg_)
                                bu_ = self.pb.get()
                                self.gemm(bu_, s_up, 16, o2, hb_rhs, hb_keys)
                                self.tt(mbuf[:, jl, :], s3[:], self.ps[bu_][:, :], ALU.mult, ["s3", ("ps", bu_)], [("mbuf", jl)])
                                self.pb.put(bu_)
                        for op_ in range(8):
                            s_w = self.load_panel(self.w_dn, blk0, nblk, op_ * 256, 256)
                            for o2 in range(2):
                                oc = op_ * 2 + o2
                                bank = self.pb.get()
                                self.gemm(bank, s_w, nblk, o2, lambda kc: mbuf[:, kc, :], lambda kc: [("mbuf", kc)])
                                self.tt(xT[:, oc, :], xT[:, oc, :], self.ps[bank][:, :], ALU.add, [xkey(oc), ("ps", bank)], [xkey(oc)])
                                self.pb.put(bank)
                        blk0 += nblk
                    for kc in range(16):
                        self.cp("act" if kc % 2 == 0 else "dve", hbuf[:, kc, :], xT[:, kc, :], [xkey(kc)], [("hbuf", kc // 4)])
                    self.dma("sp", pst[:], self.p_d[g * GT:(g + 1) * GT, :].rearrange("(t q) c -> q t c", q=128), [], ["pst"], "pst")
                    for ti in range(4):
                        bank = self.pb.get()
                        for j in range(2):
                            self.tr(self.ps[bank][:, j * 128:(j + 1) * 128], pst[:, ti, j * 128:(j + 1) * 128], self.ident_f,
                                    ["pst", "cst"], [("ps", bank)])
                        self.cp("dve", pTg[:, :, ti * 128:(ti + 1) * 128], self.ps[bank][:, 0:256].rearrange("p (j t) -> p j t", j=2),
                                [("ps", bank)], ["pTg"])
                        self.pb.put(bank)
                    for op_ in range(8):
                        s_g_ = self.load_panel(self.w_pg, 0, 16, op_ * 256, 256)
                        s_p_ = self.load_panel(self.w_ple, 0, 2, op_ * 256, 256)
                        for o2 in range(2):
                            oc = op_ * 2 + o2
                            b1_ = self.pb.get()
                            self.gemm(b1_, s_g_, 16, o2, hb_rhs, hb_keys)
                            self.act(s1[:], self.ps[b1_][:, :], AF.Sigmoid, [("ps", b1_)], ["s1"])
                            self.pb.put(b1_)
                            b2_ = self.pb.get()
                            self.gemm(b2_, s_p_, 2, o2, lambda kc: pTg[:, kc, :], lambda kc: ["pTg"])
                            self.tt(s1[:], self.ps[b2_][:, :], s1[:], ALU.mult, [("ps", b2_), "s1"], ["s1"])
                            self.pb.put(b2_)
                            self.tt(xT[:, oc, :], xT[:, oc, :], s1[:], ALU.add, [xkey(oc), "s1"], [xkey(oc)])
                    self.rmsnorm_fm(xT, g_fin, lambda kc: xT[:, kc, :], lambda kc: ("xT", kc // 4))
                    for ti in range(4):
                        b = ocount % 2
                        ocount += 1
                        for q4 in range(4):
                            bank = self.pb.get()
                            for j in range(4):
                                kc = q4 * 4 + j
                                self.tr(self.ps[bank][:, j * 128:(j + 1) * 128], xT[:, kc, ti * 128:(ti + 1) * 128], self.ident_f,
                                        [xkey(kc), "cst"], [("ps", bank)])
                            self.cp("act" if q4 % 2 == 0 else "dve", xst[b][:, q4 * 512:(q4 + 1) * 512], self.ps[bank][:, :],
                                    [("ps", bank)], [("xst", b)])
                            self.pb.put(bank)
                        r0 = (4 * g + ti) * 128
                        self.dma("sp", self.out_d[r0:r0 + 128, :], xst[b][:], [("xst", b)], [], ("ost", b))
                S.emit()
        return nc


def _t5_bucket_np(rel):
    n = np.maximum(rel, 0)
    nf = np.maximum(n, 16).astype(np.float32)
    large = 16 + (np.log(nf / np.float32(16)) / np.float32(math.log(128 / 16)) * np.float32(16)).astype(np.int32)
    large = np.minimum(large, 31)
    return np.where(n < 16, n, large)


def _constants():
    cst = np.zeros((128, 1024), np.float32)
    cst[:, 0:128] = np.eye(128, dtype=np.float32)
    pm = np.zeros((16, 8), np.float32)
    om = np.full((16, 8), -1.0, np.float32)
    for i in range(16):
        for n in range(8):
            if n >= i // 2:
                pm[i, n] = NEG
            if n == i // 2:
                om[i, n] = 0.0
    cst[:, 128:256] = pm.reshape(1, 128)
    cst[:, 256:384] = om.reshape(1, 128)
    s = np.arange(128)[:, None]
    t = np.arange(128)[None, :]
    cst[:, 384:512] = ((s // 64 == t // 64) & (s <= t)).astype(np.float32)
    rm = np.ones(512, np.float32)
    rm[0::64] = 0.0
    cst[:, 512:1024] = rm[None, :]
    oh = np.zeros((32, 512), np.float32)
    rel = np.arange(384) - 127
    bk = _t5_bucket_np(rel)
    for r in range(384):
        if rel[r] >= 0:
            oh[bk[r], r] = 1.0
    oh[31, 384:512] = 1.0
    c8 = np.zeros((8, 1408), np.float32)
    c8[:, 0:127] = -BIG
    for k in range(8):
        c8[k, 384 + k * 128:384 + (k + 1) * 128] = BIG
    return cst, oh, c8


_NC_CACHE = {}


def _get_nc(debug=False, stop=9):
    if (debug, stop) not in _NC_CACHE:
        _NC_CACHE[(debug, stop)] = Prog(debug, stop).build()
    return _NC_CACHE[(debug, stop)]


def _prep_inputs(x, p, norm_mix, w_in, hgrn_norm, w_proj_a, w_proj_b, w_out, norm_ffn, w_gate_up,
                 w_down, w_ple, w_ple_gate, rel_bias, hgrn_lb_logits, norm_final):
    f = lambda a: np.ascontiguousarray(np.asarray(a, dtype=np.float32))
    cst, oh, c8 = _constants()
    fm16 = lambda v: f(v).reshape(16, 128).T
    fm8 = lambda v: f(v).reshape(8, 128).T
    lbl = f(hgrn_lb_logits)
    vec = np.ascontiguousarray(np.concatenate(
        [fm16(norm_mix[0]), fm16(norm_ffn[0]), fm16(norm_final), fm8(hgrn_norm[0]), fm8(lbl[0]), fm8(lbl[1])], axis=1))
    shared = {
        "w_in": f(w_in[0]), "w_pa": f(w_proj_a[0]), "w_pb": f(w_proj_b[0]), "w_out": f(w_out[0]),
        "w_gu": f(w_gate_up[0]), "w_dn": f(w_down[0]), "w_ple": f(w_ple[0]), "w_pg": f(w_ple_gate[0]),
        "cst": cst, "vec": vec, "oh": oh, "c8": c8, "rb": f(rel_bias),
    }
    x = np.asarray(x)
    p = np.asarray(p)
    maps = []
    for b in range(x.shape[0]):
        m = dict(shared)
        m["x"] = f(x[b])
        m["p"] = f(p[0, b])
        maps.append(m)
    return maps


def kernel(**inputs):
    maps = _prep_inputs(**inputs)
    nc = _get_nc(False)
    res = run_bass_kernel_spmd(nc, maps, core_ids=list(range(len(maps))))
    return np.stack([np.asarray(r["out"], dtype=np.float32) for r in res.results], axis=0)
```

```python
import math
import os
import contextlib
import numpy as np
import concourse.bass as bass
import concourse.mybir as mybir
from concourse.bass_utils import run_bass_kernel_spmd

AF = mybir.ActivationFunctionType
ALU = mybir.AluOpType
AX = mybir.AxisListType
F32 = mybir.dt.float32
BF16 = mybir.dt.bfloat16

S_LEN = 2048
D = 2048
NG = 4
GT = 512
DFF = 5632
BIG = 30000.0
NEG = -1.0e30
SCALE = 1.0 / math.sqrt(128.0)
EPS = 1e-6
ENGS = ("pe", "act", "dve", "pool", "sp")


class _Op:
    __slots__ = ("eng", "fn", "deps", "dma", "semval", "flag", "sigidx", "idx")


class Sched:
    def __init__(self, nc, stack):
        self.nc = nc
        self.stack = stack
        self.ops = []
        self.last_w = {}
        self.readers = {}
        self.dma_cnt = {}
        self.cnt = {e: 0 for e in ENGS}
        self.esem = {e: stack.enter_context(nc.semaphore("s_" + e)) for e in ENGS}
        self.dsem = {}
        self.nsem = 0

    def add(self, eng, fn, reads=(), writes=(), dma=None):
        op = _Op()
        op.eng, op.fn, op.dma, op.flag, op.sigidx = eng, fn, dma, False, 0
        op.idx = len(self.ops)
        deps = {}
        for r in reads:
            p = self.last_w.get(r)
            if p is not None:
                deps[p] = "raw"
            if isinstance(r, tuple) and r[0] == "ps":
                for p in self.readers.get(r, {}).values():
                    if p not in deps:
                        deps[p] = "rar"
        for w in writes:
            p = self.last_w.get(w)
            if p is not None and p not in deps:
                deps[p] = "waw"
            for p in self.readers.get(w, {}).values():
                if p not in deps:
                    deps[p] = "war"
        op.deps = deps
        if dma is not None:
            self.dma_cnt[dma] = self.dma_cnt.get(dma, 0) + 16
            op.semval = self.dma_cnt[dma]
            if dma not in self.dsem:
                self.dsem[dma] = self.stack.enter_context(self.nc.semaphore("d%d" % self.nsem))
                self.nsem += 1
        for r in reads:
            d = self.readers.setdefault(r, {})
            d[eng if dma is None else ("dma", op.idx)] = op.idx
        for w in writes:
            self.last_w[w] = op.idx
            self.readers[w] = {}
        self.ops.append(op)
        return op

    def emit(self):
        nc = self.nc
        ops = self.ops
        waits = []
        for op in ops:
            wl = []
            for p, kind in op.deps.items():
                P = ops[p]
                if P.dma is not None:
                    wl.append(("dma", P.dma, P.semval))
                elif P.eng == op.eng and op.dma is None:
                    if P.eng == "pe" or kind != "raw":
                        continue
                    P.flag = True
                    wl.append(("eng", p))
                else:
                    P.flag = True
                    wl.append(("eng", p))
            waits.append(wl)
        last = {}
        for op in ops:
            if op.dma is None and op.fn is not None:
                last[op.eng] = op
        for op in last.values():
            op.flag = True
        for op in ops:
            if op.flag:
                self.cnt[op.eng] += 1
                op.sigidx = self.cnt[op.eng]
        esem, dsem = self.esem, self.dsem
        endcnt = dict(self.cnt)
        enddma = dict(self.dma_cnt)
        with nc.Block() as block:
            def run(ename):
                def body(eng):
                    waited = {}
                    for op, wl in zip(ops, waits):
                        if op.eng != ename:
                            continue
                        need = {}
                        for w in wl:
                            if w[0] == "dma":
                                s, v = dsem[w[1]], w[2]
                            else:
                                P = ops[w[1]]
                                s, v = esem[P.eng], P.sigidx
                            if need.get(s, 0) < v:
                                need[s] = v
                        for s, v in need.items():
                            if waited.get(s, 0) < v:
                                eng.wait_ge(s, v)
                                waited[s] = v
                        ins = op.fn(eng)
                        if op.dma is not None:
                            ins.then_inc(dsem[op.dma], 16)
                        elif op.flag:
                            ins.then_inc(esem[ename], 1)
                    for e2 in ENGS:
                        if endcnt[e2] > 0:
                            eng.wait_ge(esem[e2], endcnt[e2])
                    for k, v in enddma.items():
                        eng.wait_ge(dsem[k], v)
                return body

            block.tensor(run("pe"))
            block.scalar(run("act"))
            block.vector(run("dve"))
            block.gpsimd(run("pool"))
            block.sync(run("sp"))
        self.ops = []
        self.last_w = {}
        self.readers = {}


class Banks:
    def __init__(self, n):
        self.free = list(range(n))

    def get(self):
        return self.free.pop(0)

    def put(self, b):
        self.free.append(b)


class Prog:
    def __init__(self, debug=False, stop=9):
        self.debug = debug
        self.stop = stop
        self.nc = bass.Bass("TRN2", target_bir_lowering=False)

    def mm(self, out, lhsT, rhs, start, stop, reads, writes):
        self.S.add("pe", lambda e: e.matmul(out, lhsT=lhsT, rhs=rhs, start=start, stop=stop), reads, writes)

    def tr(self, out, in_, ident, reads, writes):
        self.S.add("pe", lambda e: e.transpose(out=out, in_=in_, identity=ident), reads, writes)

    def act(self, out, in_, func, reads, writes, **kw):
        self.S.add("act", lambda e: e.activation(out=out, in_=in_, func=func, **kw), reads, writes)

    def tt(self, out, in0, in1, op, reads, writes):
        self.S.add("dve", lambda e: e.tensor_tensor(out=out, in0=in0, in1=in1, op=op), reads, writes)

    def ts(self, out, in0, s1, s2, op0, op1, reads, writes):
        self.S.add("dve", lambda e: e.tensor_scalar(out=out, in0=in0, scalar1=s1, scalar2=s2, op0=op0, op1=op1), reads, writes)

    def stt(self, out, in0, scalar, in1, op0, op1, reads, writes):
        self.S.add("dve", lambda e: e.scalar_tensor_tensor(out=out, in0=in0, scalar=scalar, in1=in1, op0=op0, op1=op1), reads, writes)

    def cp(self, eng, out, in_, reads, writes):
        if eng == "act":
            self.S.add("act", lambda e: e.activation(out=out, in_=in_, func=AF.Copy), reads, writes)
        else:
            self.S.add("dve", lambda e: e.tensor_copy(out=out, in_=in_), reads, writes)

    def recip(self, out, in_, reads, writes):
        self.S.add("dve", lambda e: e.reciprocal(out=out, in_=in_), reads, writes)

    def dma(self, q, out, in_, reads, writes, key):
        self.S.add(q, lambda e: e.dma_start(out=out, in_=in_), reads, writes, dma=key)

    def load_panel(self, wd, k0, KC, c0, W):
        s = self.wnext % len(self.wring)
        self.wnext += 1
        src = wd[k0 * 128:(k0 + KC) * 128, c0:c0 + W].rearrange("(kc p) n -> p kc n", p=128)
        dst = self.wring[s][:, 0:KC, 0:W]
        self.dma("pool", dst, src, [], [("w", s)], ("w", s))
        return s

    def gemm(self, bank, slot, KC, oc, rhs_fn, rhs_keys):
        for kc in range(KC):
            self.mm(self.ps[bank][:, :], self.wring[slot][:, kc, oc * 128:(oc + 1) * 128], rhs_fn(kc),
                    kc == 0, kc == KC - 1, [("w", slot)] + rhs_keys(kc), [("ps", bank)])

    def load_xT(self, g, xT, xst, kp="xT"):
        for ti in range(4):
            i = 4 * g + ti
            b = self.xcnt % 2
            self.xcnt += 1
            self.dma("sp", xst[b][:], self.x_d[i * 128:(i + 1) * 128, :], [], [("xst", b)], ("xst", b))
            for q4 in range(4):
                bank = self.pb.get()
                for j in range(4):
                    kc = q4 * 4 + j
                    self.tr(self.ps[bank][:, j * 128:(j + 1) * 128], xst[b][:, kc * 128:(kc + 1) * 128], self.ident_f,
                            [("xst", b), "cst"], [("ps", bank)])
                self.cp("act" if q4 % 2 == 0 else "dve",
                        xT[:, q4 * 4:q4 * 4 + 4, ti * 128:(ti + 1) * 128],
                        self.ps[bank][:, :].rearrange("p (j t) -> p j t", j=4),
                        [("ps", bank)], [(kp, q4)])
                self.pb.put(bank)

    def rmsnorm_fm(self, src, gain, dst_fn, dstkey, nfeat=2048.0, kp="xT"):
        bank = self.pb.get()
        for kc in range(16):
            b = self.sqc % 2
            self.sqc += 1
            self.act(self.sq[b][:], src[:, kc, :], AF.Square, [(kp, kc // 4)], [("sq", b)])
            self.mm(self.ps[bank][:, :], self.ones_b[:], self.sq[b][:], kc == 0, kc == 15,
                    [("sq", b), "ones"], [("ps", bank)])
        self.act(self.rt[:], self.ps[bank][:, :], AF.Sqrt, [("ps", bank), "eps"], ["rt"], scale=1.0 / nfeat, bias=self.eps[:, 0:1])
        self.pb.put(bank)
        self.recip(self.rstd[:], self.rt[:], ["rt"], ["rstd"])
        for kc in range(16):
            self.stt(dst_fn(kc), src[:, kc, :], gain[:, kc:kc + 1], self.rstd[:], ALU.mult, ALU.mult,
                     [(kp, kc // 4), "rstd", "vec"], [dstkey(kc)])

    def build(self):
        nc = self.nc
        dti = lambda name, shape: nc.dram_tensor(name, shape, F32, kind="ExternalInput").ap()
        self.x_d = dti("x", [S_LEN, D])
        self.p_d = dti("p", [S_LEN, 256])
        self.w_in = dti("w_in", [D, 11264])
        self.w_pa = dti("w_pa", [1024, D])
        self.w_pb = dti("w_pb", [1024, D])
        self.w_out = dti("w_out", [D, D])
        self.w_gu = dti("w_gu", [D, 2 * DFF])
        self.w_dn = dti("w_dn", [DFF, D])
        self.w_ple = dti("w_ple", [256, D])
        self.w_pg = dti("w_pg", [D, D])
        cst_d = dti("cst", [128, 1024])
        vec_d = dti("vec", [128, 72])
        oh_d = dti("oh", [32, 512])
        c8_d = dti("c8", [8, 1408])
        rb_d = dti("rb", [32, 8])
        self.out_d = nc.dram_tensor("out", [S_LEN, D], F32, kind="ExternalOutput").ap()
        tz = nc.dram_tensor("tz", [8, 128, 384], F32)
        if self.debug:
            self.dbg_hT = nc.dram_tensor("dbg_hT", [128, 16 * 2048], BF16, kind="ExternalOutput").ap()
            self.dbg_ya = nc.dram_tensor("dbg_ya", [128, 8 * 2048], BF16, kind="ExternalOutput").ap()
            self.dbg_yb = nc.dram_tensor("dbg_yb", [128, 8 * 2048], BF16, kind="ExternalOutput").ap()
            self.dbg_bias = nc.dram_tensor("dbg_bias", [128, 8 * 2 * 128], BF16, kind="ExternalOutput").ap()

        with contextlib.ExitStack() as G:
            T = lambda st, name, shape, d: st.enter_context(nc.sbuf_tensor(name, shape, d))
            self.S = Sched(nc, G)
            self.ps = [G.enter_context(nc.psum_tensor("ps%d" % i, [128, 512], F32)) for i in range(8)]
            self.pb = Banks(8)
            self.xcnt = 0
            self.sqc = 0
            self.wnext = 0
            cst = T(G, "cst_sb", [128, 1024], F32)
            vec = T(G, "vec_sb", [128, 72], F32)
            self.ident_f = cst[:, 0:128]
            pastm = cst[:, 128:256]
            ownm = cst[:, 256:384]
            maskCT = cst[:, 384:512]
            resetm = cst[:, 512:1024]
            g_mix, g_ffn, g_fin = vec[:, 0:16], vec[:, 16:32], vec[:, 32:48]
            hg = vec[:, 48:56]
            self.ident_b = T(G, "ident_b", [128, 128], BF16)
            self.ones_b = T(G, "ones_b", [128, 128], BF16)
            eselb = T(G, "eselb", [8, 1024], BF16)
            biasT = T(G, "biasT", [128, 8, 2, 128], BF16)
            c31 = T(G, "c31", [128, 8], F32)
            lb = T(G, "lb", [128, 8], F32)
            omlb = T(G, "omlb", [128, 8], F32)
            dl = T(G, "dl", [128, 8], F32)
            self.eps = T(G, "eps", [128, 1], F32)
            self.sq = [T(G, "sq%d" % i, [128, 512], BF16) for i in range(2)]
            self.rt = T(G, "rt", [128, 512], F32)
            self.rstd = T(G, "rstd", [128, 512], F32)
            yaT = T(G, "yaT", [128, 8, 2048], BF16)
            ybT = T(G, "ybT", [128, 8, 2048], BF16)
            ident_b, ones_b = self.ident_b, self.ones_b
            S = self.S

            with contextlib.ExitStack() as H:
                hT = T(H, "hT", [128, 16, 2048], BF16)
                with contextlib.ExitStack() as P1:
                    oh = T(P1, "oh_sb", [32, 512], F32)
                    c8 = T(P1, "c8_sb", [8, 1408], F32)
                    rb = T(P1, "rb_sb", [32, 8], F32)
                    v_sb = T(P1, "v_sb", [8, 384], F32)
                    vp = T(P1, "vp", [8, 384], F32)
                    btf = T(P1, "btf", [128, 8, 2, 128], F32)
                    self.dma("sp", cst[:], cst_d, [], ["cst"], "c_cst")
                    self.dma("sp", vec[:], vec_d, [], ["vec"], "c_vec")
                    self.dma("sp", oh[:], oh_d, [], ["oh"], "c_oh")
                    self.dma("sp", c8[:], c8_d, [], ["c8"], "c_c8")
                    self.dma("sp", rb[:], rb_d, [], ["rb"], "c_rb")
                    self.cp("dve", ident_b[:], self.ident_f, ["cst"], ["identb"])
                    S.add("dve", lambda e: e.memset(ones_b[:], 1.0), [], ["ones"])
                    S.add("dve", lambda e: e.memset(self.eps[:], EPS), [], ["eps"])
                    self.cp("dve", eselb[:], c8[:, 384:1408], ["c8"], ["eselb"])
                    self.tt(dl[:], vec[:, 56:64], vec[:, 64:72], ALU.subtract, ["vec"], ["dl"])
                    self.act(lb[:], dl[:], AF.Sigmoid, ["dl"], ["lb"])
                    self.act(omlb[:], dl[:], AF.Sigmoid, ["dl"], ["omlb"], scale=-1.0)
                    b1 = self.pb.get()
                    self.mm(self.ps[b1][0:8, 0:384], rb[:, :], oh[:, 0:384], True, True, ["rb", "oh"], [("ps", b1)])
                    self.cp("act", v_sb[:], self.ps[b1][0:8, 0:384], [("ps", b1)], ["v_sb"])
                    self.pb.put(b1)
                    b2 = self.pb.get()
                    self.mm(self.ps[b2][:, 0:8], oh[:, 384:512], rb[:, :], True, True, ["rb", "oh"], [("ps", b2)])
                    self.cp("act", c31[:], self.ps[b2][:, 0:8], [("ps", b2)], ["c31"])
                    self.pb.put(b2)
                    self.stt(vp[:], v_sb[:], v_sb[:, 382:383], c8[:, 0:384], ALU.subtract, ALU.add, ["v_sb", "c8"], ["vp"])
                    self.dma("sp", tz.ap(), vp[:].unsqueeze(1).broadcast_to([8, 128, 384]), ["vp"], ["tz"], "c_tzw")
                    for ty, off in ((0, 127), (1, 255)):
                        src = bass.AP(tz, off, [[383, 128], [128 * 384, 8], [1, 128]])
                        self.dma("sp", btf[:, :, ty, :], src, ["tz"], [("btf", ty)], "c_tzr%d" % ty)
                    self.cp("dve", biasT[:], btf[:], [("btf", 0), ("btf", 1)], ["biasT"])
                    if self.debug:
                        self.dma("sp", self.dbg_bias, biasT[:].rearrange("p a b c -> p (a b c)"), ["biasT"], [], "dbgb")
                    S.emit()
                    if self.stop == 0:
                        return nc
                with contextlib.ExitStack() as P1:
                    xTa = T(P1, "xT1", [128, 16, 512], F32)
                    xTb = yaT[:].bitcast(F32).rearrange("p a (b c) -> p (a b) c", c=512)
                    xbufs = [(xTa, "xTa"), (xTb, "xTb")]
                    xst = [T(P1, "xst%d" % i, [128, 2048], F32) for i in range(2)]
                    self.load_xT(0, xbufs[0][0], xst, xbufs[0][1])
                    for g in range(NG):
                        if g + 1 < NG:
                            self.load_xT(g + 1, xbufs[(g + 1) % 2][0], xst, xbufs[(g + 1) % 2][1])
                        self.rmsnorm_fm(xbufs[g % 2][0], g_mix, lambda kc, g=g: hT[:, kc, g * GT:(g + 1) * GT],
                                        lambda kc, g=g: ("hT", g), kp=xbufs[g % 2][1])
                    if self.debug:
                        self.dma("sp", self.dbg_hT, hT[:].rearrange("p a b -> p (a b)"), [("hT", g) for g in range(4)], [], "dbg0")
                    S.emit()
                    if self.stop == 1:
                        return nc

                with contextlib.ExitStack() as A:
                    self.wring = [T(A, "wrA%d" % i, [128, 16, 128], BF16) for i in range(8)]
                    self.wnext = 0
                    hrhs = lambda g: (lambda kc: hT[:, kc, g * GT:(g + 1) * GT])
                    hkeys = lambda g: (lambda kc: [("hT", g)])
                    AM = contextlib.ExitStack()
                    AM.__enter__()
                    qT = [T(AM, "qT%d" % i, [128, 512], BF16) for i in range(2)]
                    kT = T(AM, "kT", [128, 2048], BF16)
                    V = T(AM, "V", [128, 16, 128], BF16)
                    vTg = T(AM, "vTg", [128, 512], BF16)
                    kms = T(AM, "kms", [128, 8], F32)
                    kmsb = T(AM, "kmsb", [128, 8], BF16)
                    sc = T(AM, "sc", [128, 4, 8], F32)
                    top8 = T(AM, "top8", [128, 4, 8], F32)
                    sel = T(AM, "sel", [128, 4, 8], F32)
                    selm1 = T(AM, "selm1", [128, 4, 8], BF16)
                    selT = [T(AM, "selT%d" % i, [8, 512], BF16) for i in range(2)]
                    pT = [T(AM, "pT%d" % i, [128, 512], BF16) for i in range(3)]
                    rs = T(AM, "rs", [128, 512], F32)
                    S.add("dve", lambda e: e.memset(kms[:], 0.0), [], ["kms"])
                    self.cp("dve", kmsb[:], kms[:], ["kms"], ["kmsb"])
                    pc = [0]
                    sc3, sel3 = sc[:], sel[:]

                    def m_s1(h, g, slots):
                        s_q, s_k, s_v = slots
                        qb = g % 2
                        bank = self.pb.get()
                        self.gemm(bank, s_q, 16, 0, hrhs(g), hkeys(g))
                        self.act(qT[qb][:], self.ps[bank][:, :], AF.Copy, [("ps", bank)], [("qT", qb)], scale=SCALE)
                        self.pb.put(bank)
                        bank = self.pb.get()
                        self.gemm(bank, s_k, 16, 0, hrhs(g), hkeys(g))
                        self.act(kT[:, g * GT:(g + 1) * GT], self.ps[bank][:, :], AF.Copy, [("ps", bank)], [("kT", g)])
                        S.add("dve", lambda e, bank=bank, g=g: e.tensor_reduce(
                            out=kms[:, 2 * g:2 * g + 2], in_=self.ps[bank][:, :].rearrange("p (b t) -> p b t", b=2),
                            axis=AX.X, op=ALU.add), [("ps", bank)], ["kms"])
                        self.pb.put(bank)
                        self.cp("dve", kmsb[:, 2 * g:2 * g + 2], kms[:, 2 * g:2 * g + 2], ["kms"], ["kmsb"])
                        bank = self.pb.get()
                        self.gemm(bank, s_v, 16, 0, hrhs(g), hkeys(g))
                        self.act(vTg[:], self.ps[bank][:, :], AF.Copy, [("ps", bank)], ["vTg"])
                        self.pb.put(bank)
                        bank = self.pb.get()
                        pbv = self.ps[bank][:, :].bitcast(BF16)
                        for ti in range(4):
                            self.tr(pbv[:, ti * 128:(ti + 1) * 128], vTg[:, ti * 128:(ti + 1) * 128], ident_b[:],
                                    ["vTg", "identb"], [("ps", bank)])
                        self.cp("dve", V[:, 4 * g:4 * g + 4, :], pbv[:, 0:512].rearrange("p (j t) -> p j t", j=4),
                                [("ps", bank)], [("V", g)])
                        self.pb.put(bank)
                        bank = self.pb.get()
                        for ti in range(4):
                            self.mm(self.ps[bank][:, ti * 8:(ti + 1) * 8], qT[qb][:, ti * 128:(ti + 1) * 128], kmsb[:, 0:8],
                                    True, True, [("qT", qb), "kmsb"], [("ps", bank)])
                        self.tt(sc[:].rearrange("p a b -> p (a b)"), self.ps[bank][:, 0:32], pastm[:, g * 32:(g + 1) * 32], ALU.add,
                                [("ps", bank), "cst"], ["sc"])
                        self.pb.put(bank)
                        for ti in range(4):
                            S.add("dve", lambda e, ti=ti: e.max(out=top8[:, ti, :], in_=sc[:, ti, :]), ["sc"], ["top8"])
                        self.tt(sel3, sc3, top8[:, :, 2:3].broadcast_to([128, 4, 8]), ALU.is_ge, ["sc", "top8"], ["sel"])
                        self.stt(selm1[:], sel3, -1.0, ownm[:, g * 32:(g + 1) * 32].rearrange("p (a b) -> p a b", a=4),
                                 ALU.add, ALU.max, ["sel", "cst"], ["selm1"])
                        bank = self.pb.get()
                        pbv = self.ps[bank][:, :].bitcast(BF16)
                        for ti in range(4):
                            self.tr(pbv[0:8, ti * 128:(ti + 1) * 128], selm1[:, ti, :], ident_b[:], ["selm1", "identb"], [("ps", bank)])
                        self.cp("act", selT[qb][0:8, :], pbv[0:8, 0:512], [("ps", bank)], [("selT", qb)])
                        self.pb.put(bank)

                    def m_s2(h, g):
                        qb = sb = g % 2
                        bo = self.pb.get()
                        bs = self.pb.get()
                        nj = 4 * g + 4

                        def emitS(j):
                            c0 = max(0, j - 4 * g) * 128
                            bk = self.pb.get()
                            items = [(kT[:, j * 128:(j + 1) * 128], qT[qb][:, c0:512], c0, 512, [("kT", j // 4), ("qT", qb)])]
                            if j < 4 * g + 2:
                                n = j // 2
                                items.append((eselb[0:8, n * 128:(n + 1) * 128], selT[sb][0:8, c0:512], c0, 512, ["eselb", ("selT", sb)]))
                            if j >= 4 * g:
                                items.append((ident_b[:], biasT[:, h, 0, :], c0, c0 + 128, ["identb", "biasT"]))
                            if 4 * g <= j + 1 <= 4 * g + 3:
                                cs = (j + 1 - 4 * g) * 128
                                items.append((ident_b[:], biasT[:, h, 1, :], cs, cs + 128, ["identb", "biasT"]))
                            for n_, (l, r, a, b_, rd) in enumerate(items):
                                self.mm(self.ps[bk][:, a:b_], l, r, n_ == 0, n_ == len(items) - 1, rd, [("ps", bk)])
                            pbi = pc[0] % 3
                            pc[0] += 1
                            self.act(pT[pbi][:, c0:512], self.ps[bk][:, c0:512], AF.Exp, [("ps", bk), "c31"], [("pT", pbi)],
                                     bias=c31[:, h:h + 1])
                            self.pb.put(bk)
                            return pbi, c0

                        def emitPV(j, pbi, c0):
                            self.mm(self.ps[bo][:, c0:512], V[:, j, :], pT[pbi][:, c0:512], j == 0, j == nj - 1,
                                    [("V", j // 4), ("pT", pbi)], [("ps", bo)])
                            self.mm(self.ps[bs][:, c0:512], ones_b[:], pT[pbi][:, c0:512], j == 0, j == nj - 1,
                                    ["ones", ("pT", pbi)], [("ps", bs)])

                        prev = emitS(0)
                        for j in range(nj):
                            nxt = emitS(j + 1) if j + 1 < nj else None
                            emitPV(j, *prev)
                            prev = nxt
                        self.recip(rs[:], self.ps[bs][:, :], [("ps", bs)], ["rs"])
                        self.tt(yaT[:, h, g * GT:(g + 1) * GT], self.ps[bo][:, :], rs[:], ALU.mult, [("ps", bo), "rs"], [("ya", h)])
                        self.pb.put(bo)
                        self.pb.put(bs)

                    for h in range(8):
                        slots = (self.load_panel(self.w_in, 0, 16, h * 128, 128),
                                 self.load_panel(self.w_in, 0, 16, 1024 + h * 128, 128),
                                 self.load_panel(self.w_in, 0, 16, 2048 + h * 128, 128))
                        m_s1(h, 0, slots)
                        for g in range(NG):
                            if g + 1 < NG:
                                m_s1(h, g + 1, slots)
                            m_s2(h, g)
                    if self.debug:
                        self.dma("sp", self.dbg_ya, yaT[:].rearrange("p a b -> p (a b)"), [("ya", h) for h in range(8)], [], "dbg1")
                    S.emit()
                    AM.__exit__(None, None, None)
                    if self.stop == 2:
                        return nc
                    AH = contextlib.ExitStack()
                    AH.__enter__()
                    t1 = T(AH, "t1", [128, 512], F32)
                    t2 = T(AH, "t2", [128, 512], F32)
                    t3 = T(AH, "t3", [128, 512], F32)
                    t4 = T(AH, "t4", [128, 512], F32)
                    kd = [T(AH, "kd%d" % i, [128, 512], BF16) for i in range(2)]
                    ke = [T(AH, "ke%d" % i, [128, 512], BF16) for i in range(2)]
                    qd = [T(AH, "qd%d" % i, [128, 512], BF16) for i in range(2)]
                    ibT = [T(AH, "ibT%d" % i, [128, 512], BF16) for i in range(2)]
                    sg = [T(AH, "sg%d" % i, [128, 512], BF16) for i in range(2)]
                    dec = [T(AH, "dec%d" % i, [128, 8], F32) for i in range(2)]
                    ke_tm = T(AH, "ke_tm", [128, 4, 128], BF16)
                    v_tm = T(AH, "v_tm", [128, 4, 128], BF16)
                    Sf = [T(AH, "Sf%d" % i, [128, 128], F32) for i in range(2)]
                    Sbf = T(AH, "Sbf", [128, 9, 128], BF16)
                    aT = T(AH, "aT", [128, 4, 128], BF16)
                    sqb = T(AH, "sqb", [128, 512], BF16)
                    tmp = T(AH, "tmpA", [128, 512], F32)
                    v3 = lambda t: t[:].rearrange("p (c t) -> p c t", t=64)
                    hslots = {}

                    def h_s1(h, g, par):
                        if g == 0:
                            hslots[h] = (self.load_panel(self.w_in, 0, 16, 3072 + h * 128, 128),
                                         self.load_panel(self.w_in, 0, 16, 4096 + h * 128, 128),
                                         self.load_panel(self.w_in, 0, 16, 5120 + h * 128, 128),
                                         self.load_panel(self.w_in, 0, 16, 6144 + h * 128, 128))
                        s_q, s_f, s_i, s_g = hslots[h]
                        bank = self.pb.get()
                        self.gemm(bank, s_f, 16, 0, hrhs(g), hkeys(g))
                        self.act(t1[:], self.ps[bank][:, :], AF.Sigmoid, [("ps", bank)], ["t1"])
                        self.pb.put(bank)
                        self.ts(t1[:], t1[:], omlb[:, h:h + 1], lb[:, h:h + 1], ALU.mult, ALU.add, ["t1", "lb", "omlb"], ["t1"])
                        self.ts(t2[:], t1[:], -1.0, 1.0, ALU.mult, ALU.add, ["t1"], ["t2"])
                        self.act(t3[:], t1[:], AF.Ln, ["t1"], ["t3"])
                        S.add("dve", lambda e: e.tensor_tensor_scan(out=t4[:], data0=resetm, data1=t3[:], initial=0.0,
                                                                    op0=ALU.mult, op1=ALU.add), ["t3", "cst"], ["t4"])
                        self.act(t1[:], t4[:], AF.Exp, ["t4"], ["t1"])
                        self.act(t3[:], t4[:], AF.Exp, ["t4"], ["t3"], scale=-1.0)
                        self.tt(t2[:], t2[:], t3[:], ALU.mult, ["t2", "t3"], ["t2"])
                        self.cp("act", kd[par][:], t2[:], ["t2"], [("kd", par)])
                        self.tt(v3(ke[par]), v3(t2), v3(t1)[:, :, 63:64].broadcast_to([128, 8, 64]), ALU.mult, ["t2", "t1"], [("ke", par)])
                        self.cp("dve", dec[par][:], v3(t1)[:, :, 63], ["t1"], [("dec", par)])
                        bank = self.pb.get()
                        self.gemm(bank, s_q, 16, 0, hrhs(g), hkeys(g))
                        self.tt(qd[par][:], self.ps[bank][:, :], t1[:], ALU.mult, [("ps", bank), "t1"], [("qd", par)])
                        self.pb.put(bank)
                        bank = self.pb.get()
                        self.gemm(bank, s_i, 16, 0, hrhs(g), hkeys(g))
                        self.act(ibT[par][:], self.ps[bank][:, :], AF.Copy, [("ps", bank)], [("ibT", par)])
                        self.pb.put(bank)
                        bank = self.pb.get()
                        self.gemm(bank, s_g, 16, 0, hrhs(g), hkeys(g))
                        self.act(sg[par][:], self.ps[bank][:, :], AF.Silu, [("ps", bank)], [("sg", par)])
                        self.pb.put(bank)

                    def h_s2a(h, g, par):
                        if g == 0:
                            S.add("dve", lambda e: e.memset(Sf[0][:], 0.0), [], [("Sf", 0)])
                            S.add("dve", lambda e: e.memset(Sbf[:, 0, :], 0.0), [], [("Sbf", 0)])
                        else:
                            self.cp("act", Sbf[:, 0, :], Sbf[:, 8, :], [("Sbf", 8)], [("Sbf", 0)])
                        for srcT, dstT, key_s, key_d, eng in ((ke[par], ke_tm, ("ke", par), "ke_tm", "dve"), (ibT[par], v_tm, ("ibT", par), "v_tm", "act")):
                            bank = self.pb.get()
                            pbv = self.ps[bank][:, :].bitcast(BF16)
                            for ti in range(4):
                                self.tr(pbv[:, ti * 128:(ti + 1) * 128], srcT[:, ti * 128:(ti + 1) * 128], ident_b[:],
                                        [key_s, "identb"], [("ps", bank)])
                            self.cp(eng, dstT[:], pbv[:, 0:512].rearrange("p (j t) -> p j t", j=4), [("ps", bank)], [key_d])
                            self.pb.put(bank)
                        bu = [self.pb.get(), self.pb.get()]
                        ubank = lambda c: bu[c % 2]
                        ucol = lambda c: slice((c // 2) * 128, (c // 2 + 1) * 128)
                        for c in range(8):
                            ti, r0 = c // 2, (c % 2) * 64
                            self.mm(self.ps[ubank(c)][:, ucol(c)], ke_tm[r0:r0 + 64, ti, :], v_tm[r0:r0 + 64, ti, :],
                                    True, True, ["ke_tm", "v_tm"], [("ps", ubank(c))])
                        for c in range(8):
                            cg = 8 * g + c
                            self.stt(Sf[(cg + 1) % 2][:], Sf[cg % 2][:], dec[par][:, c:c + 1],
                                     self.ps[ubank(c)][:, ucol(c)], ALU.mult, ALU.add,
                                     [("Sf", cg % 2), ("dec", par), ("ps", ubank(c))], [("Sf", (cg + 1) % 2)])
                            self.cp("act", Sbf[:, c + 1, :], Sf[(cg + 1) % 2][:], [("Sf", (cg + 1) % 2)], [("Sbf", c + 1)])
                        self.pb.put(bu[0])
                        self.pb.put(bu[1])
                        bank = self.pb.get()
                        for ti in range(4):
                            self.mm(self.ps[bank][:, ti * 128:(ti + 1) * 128], kd[par][:, ti * 128:(ti + 1) * 128], qd[par][:, ti * 128:(ti + 1) * 128],
                                    True, True, [("kd", par), ("qd", par)], [("ps", bank)])
                        self.tt(aT[:], self.ps[bank][:, :].rearrange("p (j t) -> p j t", j=4),
                                maskCT.unsqueeze(1).broadcast_to([128, 4, 128]), ALU.mult, [("ps", bank), "cst"], ["aT"])
                        self.pb.put(bank)

                    def h_s2b(h, g, par):
                        bank = self.pb.get()
                        for ti in range(4):
                            self.mm(self.ps[bank][:, ti * 128:(ti + 1) * 128], v_tm[:, ti, :], aT[:, ti, :], True, False,
                                    ["v_tm", "aT"], [("ps", bank)])
                            for half in range(2):
                                c = 2 * ti + half
                                self.mm(self.ps[bank][:, c * 64:(c + 1) * 64], Sbf[:, c, :], qd[par][:, c * 64:(c + 1) * 64], False, half == 1,
                                        [("Sbf", c), ("qd", par)], [("ps", bank)])
                        self.act(sqb[:], self.ps[bank][:, :], AF.Square, [("ps", bank)], ["sqb"])
                        bank2 = self.pb.get()
                        self.mm(self.ps[bank2][:, :], ones_b[:], sqb[:], True, True, ["sqb", "ones"], [("ps", bank2)])
                        self.act(self.rt[:], self.ps[bank2][:, :], AF.Sqrt, [("ps", bank2), "eps"], ["rt"], scale=1.0 / 128.0, bias=self.eps[:, 0:1])
                        self.pb.put(bank2)
                        self.recip(self.rstd[:], self.rt[:], ["rt"], ["rstd"])
                        self.stt(tmp[:], self.ps[bank][:, :], hg[:, h:h + 1], self.rstd[:], ALU.mult, ALU.mult,
                                 [("ps", bank), "rstd", "vec"], ["tmpA"])
                        self.pb.put(bank)
                        self.tt(ybT[:, h, g * GT:(g + 1) * GT], tmp[:], sg[par][:], ALU.mult, ["tmpA", ("sg", par)], [("yb", h)])

                    seq = [(h, g) for h in range(8) for g in range(NG)]
                    h_s1(seq[0][0], seq[0][1], 0)
                    for n, (h, g) in enumerate(seq):
                        par = n % 2
                        h_s2a(h, g, par)
                        if n + 1 < len(seq):
                            h_s1(seq[n + 1][0], seq[n + 1][1], 1 - par)
                        h_s2b(h, g, par)
                    if self.debug:
                        self.dma("sp", self.dbg_yb, ybT[:].rearrange("p a b -> p (a b)"), [("yb", h) for h in range(8)], [], "dbg2")
                    S.emit()
                    AH.__exit__(None, None, None)
                    if self.stop == 3:
                        return nc

            with contextlib.ExitStack() as C:
                self.wring = [T(C, "wrC%d" % i, [128, 16, 256], BF16) for i in range(4)]
                self.wnext = 0
                xT = T(C, "xTc", [128, 16, 512], F32)
                hbuf = T(C, "hbuf", [128, 16, 512], BF16)
                mbuf = T(C, "mbuf", [128, 16, 512], BF16)
                xst = [T(C, "xsc%d" % i, [128, 2048], F32) for i in range(2)]
                s1 = T(C, "s1", [128, 512], F32)
                s2 = T(C, "s2", [128, 512], F32)
                s3 = T(C, "s3", [128, 512], F32)
                pst = T(C, "pst", [128, 4, 256], F32)
                pTg = T(C, "pTg", [128, 2, 512], BF16)
                xkey = lambda oc: ("xT", oc // 4)
                ocount = 0
                for g in range(NG):
                    tok = slice(g * GT, (g + 1) * GT)
                    self.load_xT(g, xT, xst)
                    self.rmsnorm_fm(xT, g_mix, lambda kc: hbuf[:, kc, :], lambda kc: ("hbuf", kc))
                    hb_rhs = lambda kc: hbuf[:, kc, :]
                    hb_keys = lambda kc: [("hbuf", kc)]
                    for op_ in range(8):
                        s_ga = self.load_panel(self.w_in, 0, 16, 7168 + op_ * 256, 256)
                        s_gb = self.load_panel(self.w_in, 0, 16, 9216 + op_ * 256, 256)
                        s_pa = self.load_panel(self.w_pa, 0, 8, op_ * 256, 256)
                        s_pb = self.load_panel(self.w_pb, 0, 8, op_ * 256, 256)
                        for o2 in range(2):
                            oc = op_ * 2 + o2
                            bga = self.pb.get()
                            self.gemm(bga, s_ga, 16, o2, hb_rhs, hb_keys)
                            self.act(s1[:], self.ps[bga][:, :], AF.Sigmoid, [("ps", bga)], ["s1"])
                            self.pb.put(bga)
                            bgb = self.pb.get()
                            self.gemm(bgb, s_gb, 16, o2, hb_rhs, hb_keys)
                            self.act(s2[:], self.ps[bgb][:, :], AF.Sigmoid, [("ps", bgb)], ["s2"])
                            self.pb.put(bgb)
                            bpa = self.pb.get()
                            self.gemm(bpa, s_pa, 8, o2, lambda kc: yaT[:, kc, tok], lambda kc: [("ya", kc)])
                            self.tt(s1[:], self.ps[bpa][:, :], s1[:], ALU.mult, [("ps", bpa), "s1"], ["s1"])
                            self.pb.put(bpa)
                            bpb = self.pb.get()
                            self.gemm(bpb, s_pb, 8, o2, lambda kc: ybT[:, kc, tok], lambda kc: [("yb", kc)])
                            self.tt(s2[:], self.ps[bpb][:, :], s2[:], ALU.mult, [("ps", bpb), "s2"], ["s2"])
                            self.pb.put(bpb)
                            self.tt(mbuf[:, oc, :], s1[:], s2[:], ALU.add, ["s1", "s2"], [("mbuf", oc)])
                    for op_ in range(8):
                        s_w = self.load_panel(self.w_out, 0, 16, op_ * 256, 256)
                        for o2 in range(2):
                            oc = op_ * 2 + o2
                            bank = self.pb.get()
                            self.gemm(bank, s_w, 16, o2, lambda kc: mbuf[:, kc, :], lambda kc: [("mbuf", kc)])
                            self.tt(xT[:, oc, :], xT[:, oc, :], self.ps[bank][:, :], ALU.add, [xkey(oc), ("ps", bank)], [xkey(oc)])
                            self.pb.put(bank)
                    self.rmsnorm_fm(xT, g_ffn, lambda kc: hbuf[:, kc, :], lambda kc: ("hbuf", kc))
                    blk0 = 0
                    for nblk in (16, 16, 12):
                        for pr in range(nblk // 2):
                            cj = blk0 + 2 * pr
                            s_gt = self.load_panel(self.w_gu, 0, 16, cj * 128, 256)
                            s_up = self.load_panel(self.w_gu, 0, 16, DFF + cj * 128, 256)
                            for o2 in range(2):
                                jl = 2 * pr + o2
                                bg_ = self.pb.get()
                                self.gemm(bg_, s_gt, 16, o2, hb_rhs, hb_keys)
                                self.act(s3[:], self.ps[bg_][:, :], AF.Silu, [("ps", bg_)], ["s3"])
                                self.pb.put(bg_)
                                bu_ = self.pb.get()
                                self.gemm(bu_, s_up, 16, o2, hb_rhs, hb_keys)
                                self.tt(mbuf[:, jl, :], s3[:], self.ps[bu_][:, :], ALU.mult, ["s3", ("ps", bu_)], [("mbuf", jl)])
                                self.pb.put(bu_)
                        for op_ in range(8):
                            s_w = self.load_panel(self.w_dn, blk0, nblk, op_ * 256, 256)
                            for o2 in range(2):
                                oc = op_ * 2 + o2
                                bank = self.pb.get()
                                self.gemm(bank, s_w, nblk, o2, lambda kc: mbuf[:, kc, :], lambda kc: [("mbuf", kc)])
                                self.tt(xT[:, oc, :], xT[:, oc, :], self.ps[bank][:, :], ALU.add, [xkey(oc), ("ps", bank)], [xkey(oc)])
                                self.pb.put(bank)
                        blk0 += nblk
                    for kc in range(16):
                        self.cp("act" if kc % 2 == 0 else "dve", hbuf[:, kc, :], xT[:, kc, :], [xkey(kc)], [("hbuf", kc)])
                    self.dma("sp", pst[:], self.p_d[g * GT:(g + 1) * GT, :].rearrange("(t q) c -> q t c", q=128), [], ["pst"], "pst")
                    for ti in range(4):
                        bank = self.pb.get()
                        for j in range(2):
                            self.tr(self.ps[bank][:, j * 128:(j + 1) * 128], pst[:, ti, j * 128:(j + 1) * 128], self.ident_f,
                                    ["pst", "cst"], [("ps", bank)])
                        self.cp("dve", pTg[:, :, ti * 128:(ti + 1) * 128], self.ps[bank][:, 0:256].rearrange("p (j t) -> p j t", j=2),
                                [("ps", bank)], ["pTg"])
                        self.pb.put(bank)
                    for op_ in range(8):
                        s_g_ = self.load_panel(self.w_pg, 0, 16, op_ * 256, 256)
                        s_p_ = self.load_panel(self.w_ple, 0, 2, op_ * 256, 256)
                        for o2 in range(2):
                            oc = op_ * 2 + o2
                            b1_ = self.pb.get()
                            self.gemm(b1_, s_g_, 16, o2, hb_rhs, hb_keys)
                            self.act(s1[:], self.ps[b1_][:, :], AF.Sigmoid, [("ps", b1_)], ["s1"])
                            self.pb.put(b1_)
                            b2_ = self.pb.get()
                            self.gemm(b2_, s_p_, 2, o2, lambda kc: pTg[:, kc, :], lambda kc: ["pTg"])
                            self.tt(s1[:], self.ps[b2_][:, :], s1[:], ALU.mult, [("ps", b2_), "s1"], ["s1"])
                            self.pb.put(b2_)
                            self.tt(xT[:, oc, :], xT[:, oc, :], s1[:], ALU.add, [xkey(oc), "s1"], [xkey(oc)])
                    self.rmsnorm_fm(xT, g_fin, lambda kc: xT[:, kc, :], lambda kc: ("xT", kc // 4))
                    for ti in range(4):
                        b = ocount % 2
                        ocount += 1
                        for q4 in range(4):
                            bank = self.pb.get()
                            for j in range(4):
                                kc = q4 * 4 + j
                                self.tr(self.ps[bank][:, j * 128:(j + 1) * 128], xT[:, kc, ti * 128:(ti + 1) * 128], self.ident_f,
                                        [xkey(kc), "cst"], [("ps", bank)])
                            self.cp("act" if q4 % 2 == 0 else "dve", xst[b][:, q4 * 512:(q4 + 1) * 512], self.ps[bank][:, :],
                                    [("ps", bank)], [("xst", b)])
                            self.pb.put(bank)
                        r0 = (4 * g + ti) * 128
                        self.dma("sp", self.out_d[r0:r0 + 128, :], xst[b][:], [("xst", b)], [], ("ost", b))
                S.emit()
        return nc


def _t5_bucket_np(rel):
    n = np.maximum(rel, 0)
    nf = np.maximum(n, 16).astype(np.float32)
    large = 16 + (np.log(nf / np.float32(16)) / np.float32(math.log(128 / 16)) * np.float32(16)).astype(np.int32)
    large = np.minimum(large, 31)
    return np.where(n < 16, n, large)


def _constants():
    cst = np.zeros((128, 1024), np.float32)
    cst[:, 0:128] = np.eye(128, dtype=np.float32)
    pm = np.zeros((16, 8), np.float32)
    om = np.full((16, 8), -1.0, np.float32)
    for i in range(16):
        for n in range(8):
            if n >= i // 2:
                pm[i, n] = NEG
            if n == i // 2:
                om[i, n] = 0.0
    cst[:, 128:256] = pm.reshape(1, 128)
    cst[:, 256:384] = om.reshape(1, 128)
    s = np.arange(128)[:, None]
    t = np.arange(128)[None, :]
    cst[:, 384:512] = ((s // 64 == t // 64) & (s <= t)).astype(np.float32)
    rm = np.ones(512, np.float32)
    rm[0::64] = 0.0
    cst[:, 512:1024] = rm[None, :]
    oh = np.zeros((32, 512), np.float32)
    rel = np.arange(384) - 127
    bk = _t5_bucket_np(rel)
    for r in range(384):
        if rel[r] >= 0:
            oh[bk[r], r] = 1.0
    oh[31, 384:512] = 1.0
    c8 = np.zeros((8, 1408), np.float32)
    c8[:, 0:127] = -BIG
    for k in range(8):
        c8[k, 384 + k * 128:384 + (k + 1) * 128] = BIG
    return cst, oh, c8


_NC_CACHE = {}


def _get_nc(debug=False, stop=9):
    if (debug, stop) not in _NC_CACHE:
        _NC_CACHE[(debug, stop)] = Prog(debug, stop).build()
    return _NC_CACHE[(debug, stop)]


def _prep_inputs(x, p, norm_mix, w_in, hgrn_norm, w_proj_a, w_proj_b, w_out, norm_ffn, w_gate_up,
                 w_down, w_ple, w_ple_gate, rel_bias, hgrn_lb_logits, norm_final):
    f = lambda a: np.ascontiguousarray(np.asarray(a, dtype=np.float32))
    cst, oh, c8 = _constants()
    fm16 = lambda v: f(v).reshape(16, 128).T
    fm8 = lambda v: f(v).reshape(8, 128).T
    lbl = f(hgrn_lb_logits)
    vec = np.ascontiguousarray(np.concatenate(
        [fm16(norm_mix[0]), fm16(norm_ffn[0]), fm16(norm_final), fm8(hgrn_norm[0]), fm8(lbl[0]), fm8(lbl[1])], axis=1))
    shared = {
        "w_in": f(w_in[0]), "w_pa": f(w_proj_a[0]), "w_pb": f(w_proj_b[0]), "w_out": f(w_out[0]),
        "w_gu": f(w_gate_up[0]), "w_dn": f(w_down[0]), "w_ple": f(w_ple[0]), "w_pg": f(w_ple_gate[0]),
        "cst": cst, "vec": vec, "oh": oh, "c8": c8, "rb": f(rel_bias),
    }
    x = np.asarray(x)
    p = np.asarray(p)
    maps = []
    for b in range(x.shape[0]):
        m = dict(shared)
        m["x"] = f(x[b])
        m["p"] = f(p[0, b])
        maps.append(m)
    return maps


def kernel(**inputs):
    maps = _prep_inputs(**inputs)
    nc = _get_nc(False)
    res = run_bass_kernel_spmd(nc, maps, core_ids=list(range(len(maps))))
    return np.stack([np.asarray(r["out"], dtype=np.float32) for r in res.results], axis=0)
```

```python
import math
import os
import contextlib
import numpy as np
import concourse.bass as bass
import concourse.mybir as mybir
from concourse.bass_utils import run_bass_kernel_spmd

AF = mybir.ActivationFunctionType
ALU = mybir.AluOpType
AX = mybir.AxisListType
F32 = mybir.dt.float32
BF16 = mybir.dt.bfloat16

S_LEN = 2048
D = 2048
NG = 4
GT = 512
DFF = 5632
BIG = 30000.0
NEG = -1.0e30
SCALE = 1.0 / math.sqrt(128.0)
EPS = 1e-6
ENGS = ("pe", "act", "dve", "pool", "sp")


class _Op:
    __slots__ = ("eng", "fn", "deps", "dma", "semval", "flag", "sigidx", "idx")


class Sched:
    def __init__(self, nc, stack):
        self.nc = nc
        self.stack = stack
        self.ops = []
        self.last_w = {}
        self.readers = {}
        self.dma_cnt = {}
        self.cnt = {e: 0 for e in ENGS}
        self.esem = {e: stack.enter_context(nc.semaphore("s_" + e)) for e in ENGS}
        self.dsem = {}
        self.nsem = 0

    def add(self, eng, fn, reads=(), writes=(), dma=None):
        op = _Op()
        op.eng, op.fn, op.dma, op.flag, op.sigidx = eng, fn, dma, False, 0
        op.idx = len(self.ops)
        deps = {}
        for r in reads:
            p = self.last_w.get(r)
            if p is not None:
                deps[p] = "raw"
            if isinstance(r, tuple) and r[0] == "ps":
                for p in self.readers.get(r, {}).values():
                    if p not in deps:
                        deps[p] = "rar"
        for w in writes:
            p = self.last_w.get(w)
            if p is not None and p not in deps:
                deps[p] = "waw"
            for p in self.readers.get(w, {}).values():
                if p not in deps:
                    deps[p] = "war"
        op.deps = deps
        if dma is not None:
            self.dma_cnt[dma] = self.dma_cnt.get(dma, 0) + 16
            op.semval = self.dma_cnt[dma]
            if dma not in self.dsem:
                self.dsem[dma] = self.stack.enter_context(self.nc.semaphore("d%d" % self.nsem))
                self.nsem += 1
        for r in reads:
            d = self.readers.setdefault(r, {})
            d[eng if dma is None else ("dma", op.idx)] = op.idx
        for w in writes:
            self.last_w[w] = op.idx
            self.readers[w] = {}
        self.ops.append(op)
        return op

    def emit(self):
        nc = self.nc
        ops = self.ops
        waits = []
        for op in ops:
            wl = []
            for p, kind in op.deps.items():
                P = ops[p]
                if P.dma is not None:
                    wl.append(("dma", P.dma, P.semval))
                elif P.eng == op.eng and op.dma is None:
                    if P.eng == "pe" or kind != "raw":
                        continue
                    P.flag = True
                    wl.append(("eng", p))
                else:
                    P.flag = True
                    wl.append(("eng", p))
            waits.append(wl)
        last = {}
        for op in ops:
            if op.dma is None and op.fn is not None:
                last[op.eng] = op
        for op in last.values():
            op.flag = True
        for op in ops:
            if op.flag:
                self.cnt[op.eng] += 1
                op.sigidx = self.cnt[op.eng]
        esem, dsem = self.esem, self.dsem
        endcnt = dict(self.cnt)
        enddma = dict(self.dma_cnt)
        with nc.Block() as block:
            def run(ename):
                def body(eng):
                    waited = {}
                    for op, wl in zip(ops, waits):
                        if op.eng != ename:
                            continue
                        need = {}
                        for w in wl:
                            if w[0] == "dma":
                                s, v = dsem[w[1]], w[2]
                            else:
                                P = ops[w[1]]
                                s, v = esem[P.eng], P.sigidx
                            if need.get(s, 0) < v:
                                need[s] = v
                        for s, v in need.items():
                            if waited.get(s, 0) < v:
                                eng.wait_ge(s, v)
                                waited[s] = v
                        ins = op.fn(eng)
                        if op.dma is not None:
                            ins.then_inc(dsem[op.dma], 16)
                        elif op.flag:
                            ins.then_inc(esem[ename], 1)
                    for e2 in ENGS:
                        if endcnt[e2] > 0:
                            eng.wait_ge(esem[e2], endcnt[e2])
                    for k, v in enddma.items():
                        eng.wait_ge(dsem[k], v)
                return body

            block.tensor(run("pe"))
            block.scalar(run("act"))
            block.vector(run("dve"))
            block.gpsimd(run("pool"))
            block.sync(run("sp"))
        self.ops = []
        self.last_w = {}
        self.readers = {}


class Banks:
    def __init__(self, n):
        self.free = list(range(n))

    def get(self):
        return self.free.pop(0)

    def put(self, b):
        self.free.append(b)


class Prog:
    def __init__(self, debug=False, stop=9):
        self.debug = debug
        self.stop = stop
        self.nc = bass.Bass("TRN2", target_bir_lowering=False)

    def mm(self, out, lhsT, rhs, start, stop, reads, writes):
        self.S.add("pe", lambda e: e.matmul(out, lhsT=lhsT, rhs=rhs, start=start, stop=stop), reads, writes)

    def tr(self, out, in_, ident, reads, writes):
        self.S.add("pe", lambda e: e.transpose(out=out, in_=in_, identity=ident), reads, writes)

    def act(self, out, in_, func, reads, writes, **kw):
        self.S.add("act", lambda e: e.activation(out=out, in_=in_, func=func, **kw), reads, writes)

    def tt(self, out, in0, in1, op, reads, writes):
        self.S.add("dve", lambda e: e.tensor_tensor(out=out, in0=in0, in1=in1, op=op), reads, writes)

    def ts(self, out, in0, s1, s2, op0, op1, reads, writes):
        self.S.add("dve", lambda e: e.tensor_scalar(out=out, in0=in0, scalar1=s1, scalar2=s2, op0=op0, op1=op1), reads, writes)

    def stt(self, out, in0, scalar, in1, op0, op1, reads, writes):
        self.S.add("dve", lambda e: e.scalar_tensor_tensor(out=out, in0=in0, scalar=scalar, in1=in1, op0=op0, op1=op1), reads, writes)

    def cp(self, eng, out, in_, reads, writes):
        if eng == "act":
            self.S.add("act", lambda e: e.activation(out=out, in_=in_, func=AF.Copy), reads, writes)
        else:
            self.S.add("dve", lambda e: e.tensor_copy(out=out, in_=in_), reads, writes)

    def recip(self, out, in_, reads, writes):
        self.S.add("dve", lambda e: e.reciprocal(out=out, in_=in_), reads, writes)

    def dma(self, q, out, in_, reads, writes, key):
        self.S.add(q, lambda e: e.dma_start(out=out, in_=in_), reads, writes, dma=key)

    def load_panel(self, wd, k0, KC, c0, W):
        s = self.wnext % len(self.wring)
        self.wnext += 1
        src = wd[k0 * 128:(k0 + KC) * 128, c0:c0 + W].rearrange("(kc p) n -> p kc n", p=128)
        dst = self.wring[s][:, 0:KC, 0:W]
        self.dma("pool", dst, src, [], [("w", s)], ("w", s))
        return s

    def gemm(self, bank, slot, KC, oc, rhs_fn, rhs_keys):
        for kc in range(KC):
            self.mm(self.ps[bank][:, :], self.wring[slot][:, kc, oc * 128:(oc + 1) * 128], rhs_fn(kc),
                    kc == 0, kc == KC - 1, [("w", slot)] + rhs_keys(kc), [("ps", bank)])

    def load_xT(self, g, xT, xst, kp="xT"):
        for ti in range(4):
            i = 4 * g + ti
            b = self.xcnt % 2
            self.xcnt += 1
            self.dma("sp", xst[b][:], self.x_d[i * 128:(i + 1) * 128, :], [], [("xst", b)], ("xst", b))
            for q4 in range(4):
                bank = self.pb.get()
                for j in range(4):
                    kc = q4 * 4 + j
                    self.tr(self.ps[bank][:, j * 128:(j + 1) * 128], xst[b][:, kc * 128:(kc + 1) * 128], self.ident_f,
                            [("xst", b), "cst"], [("ps", bank)])
                self.cp("act" if q4 % 2 == 0 else "dve",
                        xT[:, q4 * 4:q4 * 4 + 4, ti * 128:(ti + 1) * 128],
                        self.ps[bank][:, :].rearrange("p (j t) -> p j t", j=4),
                        [("ps", bank)], [(kp, q4)])
                self.pb.put(bank)

    def rmsnorm_fm(self, src, gain, dst_fn, dstkey, nfeat=2048.0, kp="xT"):
        bank = self.pb.get()
        for kc in range(16):
            b = self.sqc % 2
            self.sqc += 1
            self.act(self.sq[b][:], src[:, kc, :], AF.Square, [(kp, kc // 4)], [("sq", b)])
            self.mm(self.ps[bank][:, :], self.ones_b[:], self.sq[b][:], kc == 0, kc == 15,
                    [("sq", b), "ones"], [("ps", bank)])
        self.act(self.rt[:], self.ps[bank][:, :], AF.Ln, [("ps", bank), "eps"], ["rt"], scale=1.0 / nfeat, bias=self.eps[:, 0:1])
        self.pb.put(bank)
        self.act(self.rstd[:], self.rt[:], AF.Exp, ["rt"], ["rstd"], scale=-0.5)
        for kc in range(16):
            self.stt(dst_fn(kc), src[:, kc, :], gain[:, kc:kc + 1], self.rstd[:], ALU.mult, ALU.mult,
                     [(kp, kc // 4), "rstd", "vec"], [dstkey(kc)])

    def build(self):
        nc = self.nc
        dti = lambda name, shape: nc.dram_tensor(name, shape, F32, kind="ExternalInput").ap()
        self.x_d = dti("x", [S_LEN, D])
        self.p_d = dti("p", [S_LEN, 256])
        self.w_in = dti("w_in", [D, 11264])
        self.w_pa = dti("w_pa", [1024, D])
        self.w_pb = dti("w_pb", [1024, D])
        self.w_out = dti("w_out", [D, D])
        self.w_gu = dti("w_gu", [D, 2 * DFF])
        self.w_dn = dti("w_dn", [DFF, D])
        self.w_ple = dti("w_ple", [256, D])
        self.w_pg = dti("w_pg", [D, D])
        cst_d = dti("cst", [128, 1024])
        vec_d = dti("vec", [128, 72])
        oh_d = dti("oh", [32, 512])
        c8_d = dti("c8", [8, 1408])
        rb_d = dti("rb", [32, 8])
        self.out_d = nc.dram_tensor("out", [S_LEN, D], F32, kind="ExternalOutput").ap()
        tz = nc.dram_tensor("tz", [8, 128, 384], F32)
        if self.debug:
            self.dbg_hT = nc.dram_tensor("dbg_hT", [128, 16 * 2048], BF16, kind="ExternalOutput").ap()
            self.dbg_ya = nc.dram_tensor("dbg_ya", [128, 8 * 2048], BF16, kind="ExternalOutput").ap()
            self.dbg_yb = nc.dram_tensor("dbg_yb", [128, 8 * 2048], BF16, kind="ExternalOutput").ap()
            self.dbg_bias = nc.dram_tensor("dbg_bias", [128, 8 * 2 * 128], BF16, kind="ExternalOutput").ap()

        with contextlib.ExitStack() as G:
            T = lambda st, name, shape, d: st.enter_context(nc.sbuf_tensor(name, shape, d))
            self.S = Sched(nc, G)
            self.ps = [G.enter_context(nc.psum_tensor("ps%d" % i, [128, 512], F32)) for i in range(8)]
            self.pb = Banks(8)
            self.xcnt = 0
            self.sqc = 0
            self.wnext = 0
            cst = T(G, "cst_sb", [128, 1024], F32)
            vec = T(G, "vec_sb", [128, 72], F32)
            self.ident_f = cst[:, 0:128]
            pastm = cst[:, 128:256]
            ownm = cst[:, 256:384]
            maskCT = cst[:, 384:512]
            resetm = cst[:, 512:1024]
            g_mix, g_ffn, g_fin = vec[:, 0:16], vec[:, 16:32], vec[:, 32:48]
            hg = vec[:, 48:56]
            self.ident_b = T(G, "ident_b", [128, 128], BF16)
            self.ones_b = T(G, "ones_b", [128, 128], BF16)
            eselb = T(G, "eselb", [8, 1024], BF16)
            biasT = T(G, "biasT", [128, 8, 2, 128], BF16)
            c31 = T(G, "c31", [128, 8], F32)
            lb = T(G, "lb", [128, 8], F32)
            omlb = T(G, "omlb", [128, 8], F32)
            dl = T(G, "dl", [128, 8], F32)
            self.eps = T(G, "eps", [128, 1], F32)
            self.sq = [T(G, "sq%d" % i, [128, 512], BF16) for i in range(2)]
            self.rt = T(G, "rt", [128, 512], F32)
            self.rstd = T(G, "rstd", [128, 512], F32)
            yaT = T(G, "yaT", [128, 8, 2048], BF16)
            ybT = T(G, "ybT", [128, 8, 2048], BF16)
            ident_b, ones_b = self.ident_b, self.ones_b
            S = self.S

            with contextlib.ExitStack() as H:
                hT = T(H, "hT", [128, 16, 2048], BF16)
                with contextlib.ExitStack() as P1:
                    oh = T(P1, "oh_sb", [32, 512], F32)
                    c8 = T(P1, "c8_sb", [8, 1408], F32)
                    rb = T(P1, "rb_sb", [32, 8], F32)
                    v_sb = T(P1, "v_sb", [8, 384], F32)
                    vp = T(P1, "vp", [8, 384], F32)
                    btf = T(P1, "btf", [128, 8, 2, 128], F32)
                    self.dma("sp", cst[:], cst_d, [], ["cst"], "c_cst")
                    self.dma("sp", vec[:], vec_d, [], ["vec"], "c_vec")
                    self.dma("sp", oh[:], oh_d, [], ["oh"], "c_oh")
                    self.dma("sp", c8[:], c8_d, [], ["c8"], "c_c8")
                    self.dma("sp", rb[:], rb_d, [], ["rb"], "c_rb")
                    self.cp("dve", ident_b[:], self.ident_f, ["cst"], ["identb"])
                    S.add("dve", lambda e: e.memset(ones_b[:], 1.0), [], ["ones"])
                    S.add("dve", lambda e: e.memset(self.eps[:], EPS), [], ["eps"])
                    self.cp("dve", eselb[:], c8[:, 384:1408], ["c8"], ["eselb"])
                    self.tt(dl[:], vec[:, 56:64], vec[:, 64:72], ALU.subtract, ["vec"], ["dl"])
                    self.act(lb[:], dl[:], AF.Sigmoid, ["dl"], ["lb"])
                    self.act(omlb[:], dl[:], AF.Sigmoid, ["dl"], ["omlb"], scale=-1.0)
                    b1 = self.pb.get()
                    self.mm(self.ps[b1][0:8, 0:384], rb[:, :], oh[:, 0:384], True, True, ["rb", "oh"], [("ps", b1)])
                    self.cp("act", v_sb[:], self.ps[b1][0:8, 0:384], [("ps", b1)], ["v_sb"])
                    self.pb.put(b1)
                    b2 = self.pb.get()
                    self.mm(self.ps[b2][:, 0:8], oh[:, 384:512], rb[:, :], True, True, ["rb", "oh"], [("ps", b2)])
                    self.cp("act", c31[:], self.ps[b2][:, 0:8], [("ps", b2)], ["c31"])
                    self.pb.put(b2)
                    self.stt(vp[:], v_sb[:], v_sb[:, 382:383], c8[:, 0:384], ALU.subtract, ALU.add, ["v_sb", "c8"], ["vp"])
                    self.dma("sp", tz.ap(), vp[:].unsqueeze(1).broadcast_to([8, 128, 384]), ["vp"], ["tz"], "c_tzw")
                    for ty, off in ((0, 127), (1, 255)):
                        src = bass.AP(tz, off, [[383, 128], [128 * 384, 8], [1, 128]])
                        self.dma("sp", btf[:, :, ty, :], src, ["tz"], [("btf", ty)], "c_tzr%d" % ty)
                    self.cp("dve", biasT[:], btf[:], [("btf", 0), ("btf", 1)], ["biasT"])
                    if self.debug:
                        self.dma("sp", self.dbg_bias, biasT[:].rearrange("p a b c -> p (a b c)"), ["biasT"], [], "dbgb")
                    S.emit()
                    if self.stop == 0:
                        return nc
                with contextlib.ExitStack() as P1:
                    xTa = T(P1, "xT1", [128, 16, 512], F32)
                    xTb = yaT[:].bitcast(F32).rearrange("p a (b c) -> p (a b) c", c=512)
                    xbufs = [(xTa, "xTa"), (xTb, "xTb")]
                    xst = [T(P1, "xst%d" % i, [128, 2048], F32) for i in range(2)]
                    self.load_xT(0, xbufs[0][0], xst, xbufs[0][1])
                    for g in range(NG):
                        if g + 1 < NG:
                            self.load_xT(g + 1, xbufs[(g + 1) % 2][0], xst, xbufs[(g + 1) % 2][1])
                        self.rmsnorm_fm(xbufs[g % 2][0], g_mix, lambda kc, g=g: hT[:, kc, g * GT:(g + 1) * GT],
                                        lambda kc, g=g: ("hT", g), kp=xbufs[g % 2][1])
                    if self.debug:
                        self.dma("sp", self.dbg_hT, hT[:].rearrange("p a b -> p (a b)"), [("hT", g) for g in range(4)], [], "dbg0")
                    S.emit()
                    if self.stop == 1:
                        return nc

                with contextlib.ExitStack() as A:
                    self.wring = [T(A, "wrA%d" % i, [128, 16, 128], BF16) for i in range(8)]
                    self.wnext = 0
                    hrhs = lambda g: (lambda kc: hT[:, kc, g * GT:(g + 1) * GT])
                    hkeys = lambda g: (lambda kc: [("hT", g)])
                    AM = contextlib.ExitStack()
                    AM.__enter__()
                    qT = [T(AM, "qT%d" % i, [128, 512], BF16) for i in range(2)]
                    kT = T(AM, "kT", [128, 2048], BF16)
                    V = T(AM, "V", [128, 16, 128], BF16)
                    vTg = T(AM, "vTg", [128, 512], BF16)
                    kms = T(AM, "kms", [128, 8], F32)
                    kmsb = T(AM, "kmsb", [128, 8], BF16)
                    sc = T(AM, "sc", [128, 4, 8], F32)
                    top8 = T(AM, "top8", [128, 4, 8], F32)
                    sel = T(AM, "sel", [128, 4, 8], F32)
                    selm1 = T(AM, "selm1", [128, 4, 8], BF16)
                    selT = [T(AM, "selT%d" % i, [8, 512], BF16) for i in range(2)]
                    pT = [T(AM, "pT%d" % i, [128, 512], BF16) for i in range(3)]
                    rs = T(AM, "rs", [128, 512], F32)
                    S.add("dve", lambda e: e.memset(kms[:], 0.0), [], ["kms"])
                    self.cp("dve", kmsb[:], kms[:], ["kms"], ["kmsb"])
                    pc = [0]
                    sc3, sel3 = sc[:], sel[:]

                    def m_s1(h, g, slots):
                        s_q, s_k, s_v = slots
                        qb = g % 2
                        bank = self.pb.get()
                        self.gemm(bank, s_q, 16, 0, hrhs(g), hkeys(g))
                        self.act(qT[qb][:], self.ps[bank][:, :], AF.Copy, [("ps", bank)], [("qT", qb)], scale=SCALE)
                        self.pb.put(bank)
                        bank = self.pb.get()
                        self.gemm(bank, s_k, 16, 0, hrhs(g), hkeys(g))
                        self.act(kT[:, g * GT:(g + 1) * GT], self.ps[bank][:, :], AF.Copy, [("ps", bank)], [("kT", g)])
                        S.add("dve", lambda e, bank=bank, g=g: e.tensor_reduce(
                            out=kms[:, 2 * g:2 * g + 2], in_=self.ps[bank][:, :].rearrange("p (b t) -> p b t", b=2),
                            axis=AX.X, op=ALU.add), [("ps", bank)], ["kms"])
                        self.pb.put(bank)
                        self.cp("dve", kmsb[:, 2 * g:2 * g + 2], kms[:, 2 * g:2 * g + 2], ["kms"], ["kmsb"])
                        bank = self.pb.get()
                        self.gemm(bank, s_v, 16, 0, hrhs(g), hkeys(g))
                        self.act(vTg[:], self.ps[bank][:, :], AF.Copy, [("ps", bank)], ["vTg"])
                        self.pb.put(bank)
                        bank = self.pb.get()
                        pbv = self.ps[bank][:, :].bitcast(BF16)
                        for ti in range(4):
                            self.tr(pbv[:, ti * 128:(ti + 1) * 128], vTg[:, ti * 128:(ti + 1) * 128], ident_b[:],
                                    ["vTg", "identb"], [("ps", bank)])
                        self.cp("dve", V[:, 4 * g:4 * g + 4, :], pbv[:, 0:512].rearrange("p (j t) -> p j t", j=4),
                                [("ps", bank)], [("V", g)])
                        self.pb.put(bank)
                        bank = self.pb.get()
                        for ti in range(4):
                            self.mm(self.ps[bank][:, ti * 8:(ti + 1) * 8], qT[qb][:, ti * 128:(ti + 1) * 128], kmsb[:, 0:8],
                                    True, True, [("qT", qb), "kmsb"], [("ps", bank)])
                        self.tt(sc[:].rearrange("p a b -> p (a b)"), self.ps[bank][:, 0:32], pastm[:, g * 32:(g + 1) * 32], ALU.add,
                                [("ps", bank), "cst"], ["sc"])
                        self.pb.put(bank)
                        for ti in range(4):
                            S.add("dve", lambda e, ti=ti: e.max(out=top8[:, ti, :], in_=sc[:, ti, :]), ["sc"], ["top8"])
                        self.tt(sel3, sc3, top8[:, :, 2:3].broadcast_to([128, 4, 8]), ALU.is_ge, ["sc", "top8"], ["sel"])
                        self.stt(selm1[:], sel3, -1.0, ownm[:, g * 32:(g + 1) * 32].rearrange("p (a b) -> p a b", a=4),
                                 ALU.add, ALU.max, ["sel", "cst"], ["selm1"])
                        bank = self.pb.get()
                        pbv = self.ps[bank][:, :].bitcast(BF16)
                        for ti in range(4):
                            self.tr(pbv[0:8, ti * 128:(ti + 1) * 128], selm1[:, ti, :], ident_b[:], ["selm1", "identb"], [("ps", bank)])
                        self.cp("act", selT[qb][0:8, :], pbv[0:8, 0:512], [("ps", bank)], [("selT", qb)])
                        self.pb.put(bank)

                    def m_s2(h, g):
                        qb = sb = g % 2
                        bo = self.pb.get()
                        bs = self.pb.get()
                        nj = 4 * g + 4

                        def emitS(j):
                            c0 = max(0, j - 4 * g) * 128
                            bk = self.pb.get()
                            items = [(kT[:, j * 128:(j + 1) * 128], qT[qb][:, c0:512], c0, 512, [("kT", j // 4), ("qT", qb)])]
                            if j < 4 * g + 2:
                                n = j // 2
                                items.append((eselb[0:8, n * 128:(n + 1) * 128], selT[sb][0:8, c0:512], c0, 512, ["eselb", ("selT", sb)]))
                            if j >= 4 * g:
                                items.append((ident_b[:], biasT[:, h, 0, :], c0, c0 + 128, ["identb", "biasT"]))
                            if 4 * g <= j + 1 <= 4 * g + 3:
                                cs = (j + 1 - 4 * g) * 128
                                items.append((ident_b[:], biasT[:, h, 1, :], cs, cs + 128, ["identb", "biasT"]))
                            for n_, (l, r, a, b_, rd) in enumerate(items):
                                self.mm(self.ps[bk][:, a:b_], l, r, n_ == 0, n_ == len(items) - 1, rd, [("ps", bk)])
                            pbi = pc[0] % 3
                            pc[0] += 1
                            self.act(pT[pbi][:, c0:512], self.ps[bk][:, c0:512], AF.Exp, [("ps", bk), "c31"], [("pT", pbi)],
                                     bias=c31[:, h:h + 1])
                            self.pb.put(bk)
                            return pbi, c0

                        def emitPV(j, pbi, c0):
                            self.mm(self.ps[bo][:, c0:512], V[:, j, :], pT[pbi][:, c0:512], j == 0, j == nj - 1,
                                    [("V", j // 4), ("pT", pbi)], [("ps", bo)])
                            self.mm(self.ps[bs][:, c0:512], ones_b[:], pT[pbi][:, c0:512], j == 0, j == nj - 1,
                                    ["ones", ("pT", pbi)], [("ps", bs)])

                        prev = emitS(0)
                        for j in range(nj):
                            nxt = emitS(j + 1) if j + 1 < nj else None
                            emitPV(j, *prev)
                            prev = nxt
                        self.act(rs[:], self.ps[bs][:, :], AF.Ln, [("ps", bs)], ["rs"])
                        self.act(rs[:], rs[:], AF.Exp, ["rs"], ["rs"], scale=-1.0)
                        self.tt(yaT[:, h, g * GT:(g + 1) * GT], self.ps[bo][:, :], rs[:], ALU.mult, [("ps", bo), "rs"], [("ya", h)])
                        self.pb.put(bo)
                        self.pb.put(bs)

                    for h in range(8):
                        slots = (self.load_panel(self.w_in, 0, 16, h * 128, 128),
                                 self.load_panel(self.w_in, 0, 16, 1024 + h * 128, 128),
                                 self.load_panel(self.w_in, 0, 16, 2048 + h * 128, 128))
                        m_s1(h, 0, slots)
                        for g in range(NG):
                            if g + 1 < NG:
                                m_s1(h, g + 1, slots)
                            m_s2(h, g)
                    if self.debug:
                        self.dma("sp", self.dbg_ya, yaT[:].rearrange("p a b -> p (a b)"), [("ya", h) for h in range(8)], [], "dbg1")
                    S.emit()
                    AM.__exit__(None, None, None)
                    if self.stop == 2:
                        return nc
                    AH = contextlib.ExitStack()
                    AH.__enter__()
                    t1 = T(AH, "t1", [128, 512], F32)
                    t2 = T(AH, "t2", [128, 512], F32)
                    t3 = T(AH, "t3", [128, 512], F32)
                    t4 = T(AH, "t4", [128, 512], F32)
                    kd = [T(AH, "kd%d" % i, [128, 512], BF16) for i in range(2)]
                    ke = [T(AH, "ke%d" % i, [128, 512], BF16) for i in range(2)]
                    qd = [T(AH, "qd%d" % i, [128, 512], BF16) for i in range(2)]
                    ibT = [T(AH, "ibT%d" % i, [128, 512], BF16) for i in range(2)]
                    sg = [T(AH, "sg%d" % i, [128, 512], BF16) for i in range(2)]
                    dec = [T(AH, "dec%d" % i, [128, 8], F32) for i in range(2)]
                    ke_tm = T(AH, "ke_tm", [128, 4, 128], BF16)
                    v_tm = T(AH, "v_tm", [128, 4, 128], BF16)
                    Sf = [T(AH, "Sf%d" % i, [128, 128], F32) for i in range(2)]
                    Sbf = T(AH, "Sbf", [128, 9, 128], BF16)
                    aT = T(AH, "aT", [128, 4, 128], BF16)
                    sqb = T(AH, "sqb", [128, 512], BF16)
                    tmp = T(AH, "tmpA", [128, 512], F32)
                    v3 = lambda t: t[:].rearrange("p (c t) -> p c t", t=64)
                    hslots = {}

                    def h_s1(h, g, par):
                        if g == 0:
                            hslots[h] = (self.load_panel(self.w_in, 0, 16, 3072 + h * 128, 128),
                                         self.load_panel(self.w_in, 0, 16, 4096 + h * 128, 128),
                                         self.load_panel(self.w_in, 0, 16, 5120 + h * 128, 128),
                                         self.load_panel(self.w_in, 0, 16, 6144 + h * 128, 128))
                        s_q, s_f, s_i, s_g = hslots[h]
                        bank = self.pb.get()
                        self.gemm(bank, s_f, 16, 0, hrhs(g), hkeys(g))
                        self.act(t1[:], self.ps[bank][:, :], AF.Sigmoid, [("ps", bank)], ["t1"])
                        self.pb.put(bank)
                        bankg = self.pb.get()
                        self.gemm(bankg, s_g, 16, 0, hrhs(g), hkeys(g))
                        self.act(sg[par][:], self.ps[bankg][:, :], AF.Sigmoid, [("ps", bankg)], [("sg", par)])
                        self.ts(t1[:], t1[:], omlb[:, h:h + 1], lb[:, h:h + 1], ALU.mult, ALU.add, ["t1", "lb", "omlb"], ["t1"])
                        self.ts(t2[:], t1[:], -1.0, 1.0, ALU.mult, ALU.add, ["t1"], ["t2"])
                        self.act(t3[:], t1[:], AF.Ln, ["t1"], ["t3"])
                        S.add("dve", lambda e: e.tensor_tensor_scan(out=t4[:], data0=resetm, data1=t3[:], initial=0.0,
                                                                    op0=ALU.mult, op1=ALU.add), ["t3", "cst"], ["t4"])
                        self.act(t1[:], t4[:], AF.Exp, ["t4"], ["t1"])
                        self.act(t3[:], t4[:], AF.Exp, ["t4"], ["t3"], scale=-1.0)
                        self.tt(t2[:], t2[:], t3[:], ALU.mult, ["t2", "t3"], ["t2"])
                        self.cp("act", kd[par][:], t2[:], ["t2"], [("kd", par)])
                        self.tt(v3(ke[par]), v3(t2), v3(t1)[:, :, 63:64].broadcast_to([128, 8, 64]), ALU.mult, ["t2", "t1"], [("ke", par)])
                        self.cp("dve", dec[par][:], v3(t1)[:, :, 63], ["t1"], [("dec", par)])
                        self.tt(sg[par][:], self.ps[bankg][:, :], sg[par][:], ALU.mult, [("ps", bankg), ("sg", par)], [("sg", par)])
                        self.pb.put(bankg)
                        bank = self.pb.get()
                        self.gemm(bank, s_q, 16, 0, hrhs(g), hkeys(g))
                        self.tt(qd[par][:], self.ps[bank][:, :], t1[:], ALU.mult, [("ps", bank), "t1"], [("qd", par)])
                        self.pb.put(bank)
                        bank = self.pb.get()
                        self.gemm(bank, s_i, 16, 0, hrhs(g), hkeys(g))
                        self.act(ibT[par][:], self.ps[bank][:, :], AF.Copy, [("ps", bank)], [("ibT", par)])
                        self.pb.put(bank)

                    def h_s2a(h, g, par):
                        if g == 0:
                            S.add("dve", lambda e: e.memset(Sf[0][:], 0.0), [], [("Sf", 0)])
                            S.add("dve", lambda e: e.memset(Sbf[:, 0, :], 0.0), [], [("Sbf", 0)])
                        else:
                            self.cp("act", Sbf[:, 0, :], Sbf[:, 8, :], [("Sbf", 8)], [("Sbf", 0)])
                        for srcT, dstT, key_s, key_d, eng in ((ke[par], ke_tm, ("ke", par), "ke_tm", "dve"), (ibT[par], v_tm, ("ibT", par), "v_tm", "act")):
                            bank = self.pb.get()
                            pbv = self.ps[bank][:, :].bitcast(BF16)
                            for ti in range(4):
                                self.tr(pbv[:, ti * 128:(ti + 1) * 128], srcT[:, ti * 128:(ti + 1) * 128], ident_b[:],
                                        [key_s, "identb"], [("ps", bank)])
                            self.cp(eng, dstT[:], pbv[:, 0:512].rearrange("p (j t) -> p j t", j=4), [("ps", bank)], [key_d])
                            self.pb.put(bank)
                        bu = [self.pb.get(), self.pb.get()]
                        ubank = lambda c: bu[c % 2]
                        ucol = lambda c: slice((c // 2) * 128, (c // 2 + 1) * 128)
                        for c in range(8):
                            ti, r0 = c // 2, (c % 2) * 64
                            self.mm(self.ps[ubank(c)][:, ucol(c)], ke_tm[r0:r0 + 64, ti, :], v_tm[r0:r0 + 64, ti, :],
                                    True, True, ["ke_tm", "v_tm"], [("ps", ubank(c))])
                        for c in range(8):
                            cg = 8 * g + c
                            self.stt(Sf[(cg + 1) % 2][:], Sf[cg % 2][:], dec[par][:, c:c + 1],
                                     self.ps[ubank(c)][:, ucol(c)], ALU.mult, ALU.add,
                                     [("Sf", cg % 2), ("dec", par), ("ps", ubank(c))], [("Sf", (cg + 1) % 2)])
                            self.cp("act", Sbf[:, c + 1, :], Sf[(cg + 1) % 2][:], [("Sf", (cg + 1) % 2)], [("Sbf", c + 1)])
                        self.pb.put(bu[0])
                        self.pb.put(bu[1])
                        bank = self.pb.get()
                        for ti in range(4):
                            self.mm(self.ps[bank][:, ti * 128:(ti + 1) * 128], kd[par][:, ti * 128:(ti + 1) * 128], qd[par][:, ti * 128:(ti + 1) * 128],
                                    True, True, [("kd", par), ("qd", par)], [("ps", bank)])
                        self.tt(aT[:], self.ps[bank][:, :].rearrange("p (j t) -> p j t", j=4),
                                maskCT.unsqueeze(1).broadcast_to([128, 4, 128]), ALU.mult, [("ps", bank), "cst"], ["aT"])
                        self.pb.put(bank)

                    def h_s2b(h, g, par):
                        bank = self.pb.get()
                        for ti in range(4):
                            self.mm(self.ps[bank][:, ti * 128:(ti + 1) * 128], v_tm[:, ti, :], aT[:, ti, :], True, False,
                                    ["v_tm", "aT"], [("ps", bank)])
                            for half in range(2):
                                c = 2 * ti + half
                                self.mm(self.ps[bank][:, c * 64:(c + 1) * 64], Sbf[:, c, :], qd[par][:, c * 64:(c + 1) * 64], False, half == 1,
                                        [("Sbf", c), ("qd", par)], [("ps", bank)])
                        self.act(sqb[:], self.ps[bank][:, :], AF.Square, [("ps", bank)], ["sqb"])
                        bank2 = self.pb.get()
                        self.mm(self.ps[bank2][:, :], ones_b[:], sqb[:], True, True, ["sqb", "ones"], [("ps", bank2)])
                        self.act(self.rt[:], self.ps[bank2][:, :], AF.Ln, [("ps", bank2), "eps"], ["rt"], scale=1.0 / 128.0, bias=self.eps[:, 0:1])
                        self.pb.put(bank2)
                        self.act(self.rstd[:], self.rt[:], AF.Exp, ["rt"], ["rstd"], scale=-0.5)
                        self.stt(tmp[:], self.ps[bank][:, :], hg[:, h:h + 1], self.rstd[:], ALU.mult, ALU.mult,
                                 [("ps", bank), "rstd", "vec"], ["tmpA"])
                        self.pb.put(bank)
                        self.tt(ybT[:, h, g * GT:(g + 1) * GT], tmp[:], sg[par][:], ALU.mult, ["tmpA", ("sg", par)], [("yb", h)])

                    seq = [(h, g) for h in range(8) for g in range(NG)]
                    h_s1(seq[0][0], seq[0][1], 0)
                    for n, (h, g) in enumerate(seq):
                        par = n % 2
                        h_s2a(h, g, par)
                        if n + 1 < len(seq):
                            h_s1(seq[n + 1][0], seq[n + 1][1], 1 - par)
                        h_s2b(h, g, par)
                    if self.debug:
                        self.dma("sp", self.dbg_yb, ybT[:].rearrange("p a b -> p (a b)"), [("yb", h) for h in range(8)], [], "dbg2")
                    S.emit()
                    AH.__exit__(None, None, None)
                    if self.stop == 3:
                        return nc

            with contextlib.ExitStack() as C:
                self.wring = [T(C, "wrC%d" % i, [128, 16, 256], BF16) for i in range(4)]
                self.wnext = 0
                xT = T(C, "xTc", [128, 16, 512], F32)
                hbuf = T(C, "hbuf", [128, 16, 512], BF16)
                mbuf = T(C, "mbuf", [128, 16, 512], BF16)
                xst = [T(C, "xsc%d" % i, [128, 2048], F32) for i in range(2)]
                s1 = T(C, "s1", [128, 512], F32)
                s2 = T(C, "s2", [128, 512], F32)
                s3 = T(C, "s3", [128, 512], F32)
                pst = T(C, "pst", [128, 4, 256], F32)
                pTg = T(C, "pTg", [128, 2, 512], BF16)
                xkey = lambda oc: ("xT", oc // 4)
                ocount = 0
                for g in range(NG):
                    tok = slice(g * GT, (g + 1) * GT)
                    self.load_xT(g, xT, xst)
                    self.rmsnorm_fm(xT, g_mix, lambda kc: hbuf[:, kc, :], lambda kc: ("hbuf", kc))
                    hb_rhs = lambda kc: hbuf[:, kc, :]
                    hb_keys = lambda kc: [("hbuf", kc)]
                    for op_ in range(8):
                        s_ga = self.load_panel(self.w_in, 0, 16, 7168 + op_ * 256, 256)
                        s_gb = self.load_panel(self.w_in, 0, 16, 9216 + op_ * 256, 256)
                        s_pa = self.load_panel(self.w_pa, 0, 8, op_ * 256, 256)
                        s_pb = self.load_panel(self.w_pb, 0, 8, op_ * 256, 256)
                        for o2 in range(2):
                            oc = op_ * 2 + o2
                            bga = self.pb.get()
                            self.gemm(bga, s_ga, 16, o2, hb_rhs, hb_keys)
                            self.act(s1[:], self.ps[bga][:, :], AF.Sigmoid, [("ps", bga)], ["s1"])
                            self.pb.put(bga)
                            bgb = self.pb.get()
                            self.gemm(bgb, s_gb, 16, o2, hb_rhs, hb_keys)
                            self.act(s2[:], self.ps[bgb][:, :], AF.Sigmoid, [("ps", bgb)], ["s2"])
                            self.pb.put(bgb)
                            bpa = self.pb.get()
                            self.gemm(bpa, s_pa, 8, o2, lambda kc: yaT[:, kc, tok], lambda kc: [("ya", kc)])
                            self.tt(s1[:], self.ps[bpa][:, :], s1[:], ALU.mult, [("ps", bpa), "s1"], ["s1"])
                            self.pb.put(bpa)
                            bpb = self.pb.get()
                            self.gemm(bpb, s_pb, 8, o2, lambda kc: ybT[:, kc, tok], lambda kc: [("yb", kc)])
                            self.tt(s2[:], self.ps[bpb][:, :], s2[:], ALU.mult, [("ps", bpb), "s2"], ["s2"])
                            self.pb.put(bpb)
                            self.tt(mbuf[:, oc, :], s1[:], s2[:], ALU.add, ["s1", "s2"], [("mbuf", oc)])
                    for op_ in range(8):
                        s_w = self.load_panel(self.w_out, 0, 16, op_ * 256, 256)
                        for o2 in range(2):
                            oc = op_ * 2 + o2
                            bank = self.pb.get()
                            self.gemm(bank, s_w, 16, o2, lambda kc: mbuf[:, kc, :], lambda kc: [("mbuf", kc)])
                            self.tt(xT[:, oc, :], xT[:, oc, :], self.ps[bank][:, :], ALU.add, [xkey(oc), ("ps", bank)], [xkey(oc)])
                            self.pb.put(bank)
                    self.rmsnorm_fm(xT, g_ffn, lambda kc: hbuf[:, kc, :], lambda kc: ("hbuf", kc))
                    blk0 = 0
                    for nblk in (16, 16, 12):
                        for pr in range(nblk // 2):
                            cj = blk0 + 2 * pr
                            s_gt = self.load_panel(self.w_gu, 0, 16, cj * 128, 256)
                            s_up = self.load_panel(self.w_gu, 0, 16, DFF + cj * 128, 256)
                            for o2 in range(2):
                                jl = 2 * pr + o2
                                bg_ = self.pb.get()
                                self.gemm(bg_, s_gt, 16, o2, hb_rhs, hb_keys)
                                self.act(s3[:], self.ps[bg_][:, :], AF.Silu, [("ps", bg_)], ["s3"])
                                self.pb.put(bg_)
                                bu_ = self.pb.get()
                                self.gemm(bu_, s_up, 16, o2, hb_rhs, hb_keys)
                                self.tt(mbuf[:, jl, :], s3[:], self.ps[bu_][:, :], ALU.mult, ["s3", ("ps", bu_)], [("mbuf", jl)])
                                self.pb.put(bu_)
                        for op_ in range(8):
                            s_w = self.load_panel(self.w_dn, blk0, nblk, op_ * 256, 256)
                            for o2 in range(2):
                                oc = op_ * 2 + o2
                                bank = self.pb.get()
                                self.gemm(bank, s_w, nblk, o2, lambda kc: mbuf[:, kc, :], lambda kc: [("mbuf", kc)])
                                self.tt(xT[:, oc, :], xT[:, oc, :], self.ps[bank][:, :], ALU.add, [xkey(oc), ("ps", bank)], [xkey(oc)])
                                self.pb.put(bank)
                        blk0 += nblk
                    for kc in range(16):
                        self.cp("act" if kc % 2 == 0 else "dve", hbuf[:, kc, :], xT[:, kc, :], [xkey(kc)], [("hbuf", kc)])
                    self.dma("sp", pst[:], self.p_d[g * GT:(g + 1) * GT, :].rearrange("(t q) c -> q t c", q=128), [], ["pst"], "pst")
                    for ti in range(4):
                        bank = self.pb.get()
                        for j in range(2):
                            self.tr(self.ps[bank][:, j * 128:(j + 1) * 128], pst[:, ti, j * 128:(j + 1) * 128], self.ident_f,
                                    ["pst", "cst"], [("ps", bank)])
                        self.cp("dve", pTg[:, :, ti * 128:(ti + 1) * 128], self.ps[bank][:, 0:256].rearrange("p (j t) -> p j t", j=2),
                                [("ps", bank)], ["pTg"])
                        self.pb.put(bank)
                    for op_ in range(8):
                        s_g_ = self.load_panel(self.w_pg, 0, 16, op_ * 256, 256)
                        s_p_ = self.load_panel(self.w_ple, 0, 2, op_ * 256, 256)
                        for o2 in range(2):
                            oc = op_ * 2 + o2
                            b1_ = self.pb.get()
                            self.gemm(b1_, s_g_, 16, o2, hb_rhs, hb_keys)
                            self.act(s1[:], self.ps[b1_][:, :], AF.Sigmoid, [("ps", b1_)], ["s1"])
                            self.pb.put(b1_)
                            b2_ = self.pb.get()
                            self.gemm(b2_, s_p_, 2, o2, lambda kc: pTg[:, kc, :], lambda kc: ["pTg"])
                            self.tt(s1[:], self.ps[b2_][:, :], s1[:], ALU.mult, [("ps", b2_), "s1"], ["s1"])
                            self.pb.put(b2_)
                            self.tt(xT[:, oc, :], xT[:, oc, :], s1[:], ALU.add, [xkey(oc), "s1"], [xkey(oc)])
                    self.rmsnorm_fm(xT, g_fin, lambda kc: xT[:, kc, :], lambda kc: ("xT", kc // 4))
                    for ti in range(4):
                        b = ocount % 2
                        ocount += 1
                        for q4 in range(4):
                            bank = self.pb.get()
                            for j in range(4):
                                kc = q4 * 4 + j
                                self.tr(self.ps[bank][:, j * 128:(j + 1) * 128], xT[:, kc, ti * 128:(ti + 1) * 128], self.ident_f,
                                        [xkey(kc), "cst"], [("ps", bank)])
                            self.cp("act" if q4 % 2 == 0 else "dve", xst[b][:, q4 * 512:(q4 + 1) * 512], self.ps[bank][:, :],
                                    [("ps", bank)], [("xst", b)])
                            self.pb.put(bank)
                        r0 = (4 * g + ti) * 128
                        self.dma("sp", self.out_d[r0:r0 + 128, :], xst[b][:], [("xst", b)], [], ("ost", b))
                S.emit()
        return nc


def _t5_bucket_np(rel):
    n = np.maximum(rel, 0)
    nf = np.maximum(n, 16).astype(np.float32)
    large = 16 + (np.log(nf / np.float32(16)) / np.float32(math.log(128 / 16)) * np.float32(16)).astype(np.int32)
    large = np.minimum(large, 31)
    return np.where(n < 16, n, large)


def _constants():
    cst = np.zeros((128, 1024), np.float32)
    cst[:, 0:128] = np.eye(128, dtype=np.float32)
    pm = np.zeros((16, 8), np.float32)
    om = np.full((16, 8), -1.0, np.float32)
    for i in range(16):
        for n in range(8):
            if n >= i // 2:
                pm[i, n] = NEG
            if n == i // 2:
                om[i, n] = 0.0
    cst[:, 128:256] = pm.reshape(1, 128)
    cst[:, 256:384] = om.reshape(1, 128)
    s = np.arange(128)[:, None]
    t = np.arange(128)[None, :]
    cst[:, 384:512] = ((s // 64 == t // 64) & (s <= t)).astype(np.float32)
    rm = np.ones(512, np.float32)
    rm[0::64] = 0.0
    cst[:, 512:1024] = rm[None, :]
    oh = np.zeros((32, 512), np.float32)
    rel = np.arange(384) - 127
    bk = _t5_bucket_np(rel)
    for r in range(384):
        if rel[r] >= 0:
            oh[bk[r], r] = 1.0
    oh[31, 384:512] = 1.0
    c8 = np.zeros((8, 1408), np.float32)
    c8[:, 0:127] = -BIG
    for k in range(8):
        c8[k, 384 + k * 128:384 + (k + 1) * 128] = BIG
    return cst, oh, c8


_NC_CACHE = {}


def _get_nc(debug=False, stop=9):
    if (debug, stop) not in _NC_CACHE:
        _NC_CACHE[(debug, stop)] = Prog(debug, stop).build()
    return _NC_CACHE[(debug, stop)]


def _prep_inputs(x, p, norm_mix, w_in, hgrn_norm, w_proj_a, w_proj_b, w_out, norm_ffn, w_gate_up,
                 w_down, w_ple, w_ple_gate, rel_bias, hgrn_lb_logits, norm_final):
    f = lambda a: np.ascontiguousarray(np.asarray(a, dtype=np.float32))
    cst, oh, c8 = _constants()
    fm16 = lambda v: f(v).reshape(16, 128).T
    fm8 = lambda v: f(v).reshape(8, 128).T
    lbl = f(hgrn_lb_logits)
    vec = np.ascontiguousarray(np.concatenate(
        [fm16(norm_mix[0]), fm16(norm_ffn[0]), fm16(norm_final), fm8(hgrn_norm[0]), fm8(lbl[0]), fm8(lbl[1])], axis=1))
    shared = {
        "w_in": f(w_in[0]), "w_pa": f(w_proj_a[0]), "w_pb": f(w_proj_b[0]), "w_out": f(w_out[0]),
        "w_gu": f(w_gate_up[0]), "w_dn": f(w_down[0]), "w_ple": f(w_ple[0]), "w_pg": f(w_ple_gate[0]),
        "cst": cst, "vec": vec, "oh": oh, "c8": c8, "rb": f(rel_bias),
    }
    x = np.asarray(x)
    p = np.asarray(p)
    maps = []
    for b in range(x.shape[0]):
        m = dict(shared)
        m["x"] = f(x[b])
        m["p"] = f(p[0, b])
        maps.append(m)
    return maps


def kernel(**inputs):
    maps = _prep_inputs(**inputs)
    nc = _get_nc(False)
    res = run_bass_kernel_spmd(nc, maps, core_ids=list(range(len(maps))))
    return np.stack([np.asarray(r["out"], dtype=np.float32) for r in res.results], axis=0)
```
